# Optimizing a Trainium2 kernel written in Bass

```python
import math
import jax, jax.numpy as jnp
from jax import lax
import numpy as np

D_MODEL = 2048
BATCH = 32
SEQ = 256
DEPTH = 2
DEC_BATCH = 4
DEC_SEQ = 2048
PAST_LEN = 512

GRID_W = 64
HEAD_DIM = 128
A_HEADS = D_MODEL // 4 // HEAD_DIM
A_DK = HEAD_DIM
A_DV = HEAD_DIM
MIX_A = A_HEADS * A_DV
A_COLS = 4 * MIX_A + 4 * A_HEADS
CONV_K = 5
CHUNK = 64
B_HEADS = D_MODEL // 4 // HEAD_DIM
B_QK = HEAD_DIM // 2
B_V = HEAD_DIM
MIX_B = B_HEADS * B_V
B_COLS = 3 * MIX_B
C_HEADS = D_MODEL // 2 // HEAD_DIM
C_KV = C_HEADS // 4
C_DIM = HEAD_DIM
MIX_C = C_HEADS * C_DIM
C_COLS = (C_HEADS + 2 * C_KV) * C_DIM
IN_COLS = A_COLS + B_COLS + C_COLS
MIX_WIDTH = MIX_A + MIX_B + MIX_C
Q_BLOCK = 128
ROPE_THETA = 10000.0
N_GROUPS = 4
EXPERTS_PER_GROUP = 4
N_EXPERTS = N_GROUPS * EXPERTS_PER_GROUP
EXPERT_FF = D_MODEL // 2
TOP_K = 2
DN_ALPHA = (2 * DEPTH) ** 0.25
DN_BETA = (8 * DEPTH) ** -0.25
EPS = 1e-6

kernel_name = 'hybrid_deltanet_diffattn_gqa_hmoe_step'


def layer_norm(x, g, b):
    xf = x.astype(jnp.float32)
    mu = jnp.mean(xf, -1, keepdims=True)
    var = jnp.mean(jnp.square(xf - mu), -1, keepdims=True)
    return ((xf - mu) * lax.rsqrt(var + EPS)).astype(x.dtype) * g + b


def rms_norm(x, w):
    xf = x.astype(jnp.float32)
    return (xf * lax.rsqrt(jnp.mean(xf * xf, -1, keepdims=True) + EPS)).astype(x.dtype) * w


def l2_norm(x):
    xf = x.astype(jnp.float32)
    return (xf * lax.rsqrt(jnp.sum(xf * xf, -1, keepdims=True) + EPS)).astype(x.dtype)


def to_heads(t, n):
    b, l, _ = t.shape
    return t.reshape(b, l, n, -1).transpose(0, 2, 1, 3)


def from_heads(t):
    b, n, l, d = t.shape
    return t.transpose(0, 2, 1, 3).reshape(b, l, n * d)


def grid_positions(length):
    rows = length // GRID_W
    row = jnp.repeat(jnp.arange(rows), GRID_W)
    col = jnp.tile(jnp.arange(GRID_W), rows)
    return row, col


def rope_1d(u, pos):
    m = u.shape[-1]
    freqs = ROPE_THETA ** (-jnp.arange(0, m, 2, dtype=jnp.float32) / m)
    ang = pos.astype(jnp.float32)[:, None] * freqs[None, :]
    cos = jnp.cos(ang).astype(u.dtype)
    sin = jnp.sin(ang).astype(u.dtype)
    u1, u2 = u[..., : m // 2], u[..., m // 2:]
    return jnp.concatenate([u1 * cos - u2 * sin, u1 * sin + u2 * cos], -1)


def axial_rope(x, row, col):
    half = x.shape[-1] // 2
    return jnp.concatenate([rope_1d(x[..., :half], row), rope_1d(x[..., half:], col)], -1)


def short_conv(u, w):
    out = lax.conv_general_dilated(
        u, w[:, None, :], window_strides=(1,), padding=[(CONV_K // 2, CONV_K // 2)],
        dimension_numbers=('NWC', 'WIO', 'NWC'), feature_group_count=u.shape[-1])
    return jax.nn.silu(out)


def block_attention(q, k, v):
    b, hkv, g, lq, d = q.shape
    nb = lq // Q_BLOCK
    qb = jnp.moveaxis(q.reshape(b, hkv, g, nb, Q_BLOCK, d), 3, 0)
    scale = d ** -0.5

    def one(q_blk):
        s = jnp.einsum('bhgqd,bhkd->bhgqk', q_blk, k, preferred_element_type=jnp.float32) * scale
        p = jax.nn.softmax(s, axis=-1)
        return jnp.einsum('bhgqk,bhkd->bhgqd', p.astype(v.dtype), v)

    o = lax.map(one, qb)
    return jnp.moveaxis(o, 0, 3).reshape(b, hkv, g, lq, -1)


def gated_delta_chunked(q, k, v, beta, g, s0):
    b, h, l, _ = q.shape
    dv = v.shape[-1]
    n = l // CHUNK
    f32 = jnp.float32
    q, k, v = (t.astype(f32).reshape(b, h, n, CHUNK, -1) for t in (q, k, v))
    beta, g = (t.astype(f32).reshape(b, h, n, CHUNK) for t in (beta, g))
    gc = jnp.cumsum(g, axis=-1)
    idx = jnp.arange(CHUNK)
    incl = idx[:, None] >= idx[None, :]
    strict = idx[:, None] > idx[None, :]
    decay = jnp.where(incl, jnp.exp(jnp.where(incl, gc[..., :, None] - gc[..., None, :], 0.0)), 0.0)
    m = jnp.where(strict, beta[..., :, None] * jnp.einsum('bhncd,bhnjd->bhncj', k, k) * decay, 0.0)
    eye = jnp.broadcast_to(jnp.eye(CHUNK, dtype=f32), m.shape)
    t_inv = lax.linalg.triangular_solve(eye + m, eye, left_side=True, lower=True, unit_diagonal=True)
    u = t_inv @ (v * beta[..., None])
    w = t_inv @ (k * (beta * jnp.exp(gc))[..., None])
    qk = jnp.einsum('bhncd,bhnjd->bhncj', q, k) * decay
    q_dec = q * jnp.exp(gc)[..., None]
    k_dec = k * jnp.exp(gc[..., -1:] - gc)[..., None]
    g_end = jnp.exp(gc[..., -1])
    xs = tuple(jnp.moveaxis(t, 2, 0) for t in (u, w, qk, q_dec, k_dec, g_end))

    def step(s, inp):
        u_c, w_c, qk_c, qd_c, kd_c, ge_c = inp
        delta = u_c - w_c @ s
        o = qd_c @ s + qk_c @ delta
        s = ge_c[..., None, None] * s + jnp.swapaxes(kd_c, -1, -2) @ delta
        return s, o

    s_fin, o = lax.scan(step, s0.astype(f32), xs)
    return jnp.moveaxis(o, 0, 2).reshape(b, h, l, dv), s_fin


def mixer_a(pa, lw, s0f, s0b):
    b, l, _ = pa.shape
    qkv = short_conv(pa[..., : 3 * MIX_A], lw['a_conv'])
    q, k, v = (to_heads(t, A_HEADS) for t in jnp.split(qkv, 3, axis=-1))
    q = l2_norm(q) * (A_DK ** -0.5)
    k = l2_norm(k)
    out_gate = pa[..., 3 * MIX_A: 4 * MIX_A]
    ba = jnp.transpose(pa[..., 4 * MIX_A:].reshape(b, l, 2, 2, A_HEADS), (2, 3, 0, 4, 1))
    beta = jax.nn.sigmoid(ba[0])
    g = -jnp.exp(lw['a_log'])[:, None, :, None] * jax.nn.softplus(ba[1] + lw['a_dt_bias'][:, None, :, None])
    flip = lambda t: jnp.flip(t, axis=2)
    o_f, s_f = gated_delta_chunked(q, k, v, beta[0], g[0], s0f)
    o_b, s_b = gated_delta_chunked(flip(q), flip(k), flip(v), flip(beta[1]), flip(g[1]), s0b)
    o = (o_f + flip(o_b)).astype(pa.dtype)
    o = from_heads(rms_norm(o, lw['a_norm'])) * jax.nn.silu(out_gate)
    return o, s_f, s_b


def mixer_b(pb, lw, lam_init, pos, ctx_kv):
    q, k, v = (to_heads(t, B_HEADS) for t in jnp.split(pb, 3, axis=-1))
    if pos is not None:
        row, col = pos
        q = jnp.concatenate([axial_rope(q[..., :B_QK], row, col), axial_rope(q[..., B_QK:], row, col)], -1)
        k = jnp.concatenate([axial_rope(k[..., :B_QK], row, col), axial_rope(k[..., B_QK:], row, col)], -1)
    if ctx_kv is None:
        k_all, v_all = k, v
    else:
        k_all = jnp.concatenate([ctx_kv[:, 0], k], axis=2)
        v_all = jnp.concatenate([ctx_kv[:, 1], v], axis=2)
    o1 = block_attention(q[:, :, None, :, :B_QK], k_all[..., :B_QK], v_all)[:, :, 0]
    o2 = block_attention(q[:, :, None, :, B_QK:], k_all[..., B_QK:], v_all)[:, :, 0]
    lam_p = lw['b_lambda'].astype(jnp.float32)
    lam = jnp.exp(jnp.sum(lam_p[0] * lam_p[1])) - jnp.exp(jnp.sum(lam_p[2] * lam_p[3])) + lam_init
    o = rms_norm(o1 - lam.astype(o1.dtype) * o2, lw['b_norm']) * (1.0 - lam_init)
    return from_heads(o), k, v


def mixer_c(pc, lw, pos, ctx_kv):
    b, l, _ = pc.shape
    q, k, v = jnp.split(pc, [MIX_C, MIX_C + C_KV * C_DIM], axis=-1)
    q = rms_norm(to_heads(q, C_HEADS), lw['c_q_norm'])
    k = rms_norm(to_heads(k, C_KV), lw['c_k_norm'])
    v = to_heads(v, C_KV)
    if pos is not None:
        q = axial_rope(q, pos[0], pos[1])
        k = axial_rope(k, pos[0], pos[1])
    if ctx_kv is None:
        k_all, v_all = k, v
    else:
        k_all = jnp.concatenate([ctx_kv[:, 0], k], axis=2)
        v_all = jnp.concatenate([ctx_kv[:, 1], v], axis=2)
    o = block_attention(q.reshape(b, C_KV, C_HEADS // C_KV, l, C_DIM), k_all, v_all)
    return from_heads(o.reshape(b, C_HEADS, l, C_DIM)), k, v


def token_mixing(h, lw, lam_init, ctx):
    b, l, _ = h.shape
    proj = h @ lw['w_in']
    pa, pb, pc = jnp.split(proj, [A_COLS, A_COLS + B_COLS], axis=-1)
    if ctx is None:
        s0 = jnp.zeros((b, A_HEADS, A_DK, A_DV), jnp.float32)
        s0f, s0b, pos, kv_b, kv_c = s0, s0, None, None, None
    else:
        st_a, kv_b, kv_c = ctx
        s0f, s0b = st_a[:, 0], st_a[:, 1]
        pos = grid_positions(l)
    o_a, s_f, s_b = mixer_a(pa, lw, s0f, s0b)
    o_b, k_b, v_b = mixer_b(pb, lw, lam_init, pos, kv_b)
    o_c, k_c, v_c = mixer_c(pc, lw, pos, kv_c)
    out = jnp.concatenate([o_a, o_b, o_c], axis=-1) @ lw['w_out']
    if ctx is None:
        ctx_new = (jnp.stack([s_f, s_b], 1).astype(h.dtype), jnp.stack([k_b, v_b], 1), jnp.stack([k_c, v_c], 1))
        return out, ctx_new
    return out, None


def hier_moe(h, lw):
    b, l, d = h.shape
    x = h.reshape(b * l, d)
    gl = (x @ lw['w_group'] + lw['b_group']).astype(jnp.float32)
    gv, gi = lax.top_k(jax.nn.softmax(gl, axis=-1), 1)
    el = (x @ lw['w_router'] + lw['b_router']).astype(jnp.float32).reshape(-1, N_GROUPS, EXPERTS_PER_GROUP)
    el = jnp.take_along_axis(el, gi[:, :, None], axis=1)[:, 0]
    ev, ei = lax.top_k(el, TOP_K)
    wts = jax.nn.softmax(ev, axis=-1) * gv
    eidx = gi * EXPERTS_PER_GROUP + ei
    gates = jnp.sum(jax.nn.one_hot(eidx, N_EXPERTS, dtype=jnp.float32) * wts[..., None], axis=1).astype(h.dtype)
    a = jnp.einsum('td,edf->tef', x, lw['w_gate'])
    u = jnp.einsum('td,edf->tef', x, lw['w_up'])
    y = jnp.einsum('tef,efd->td', jax.nn.silu(a) * u * gates[:, :, None], lw['w_down'])
    return y.reshape(b, l, d)


def trunk_layer(x, cond, lw, lam_init, ctx):
    mod = (jax.nn.silu(cond) @ lw['w_ada'] + lw['b_ada'])[:, None, :]
    sh1, sc1, g1, sh2, sc2, g2 = jnp.split(mod, 6, axis=-1)
    mix, ctx_new = token_mixing(x * (1.0 + sc1) + sh1, lw, lam_init, ctx)
    x = layer_norm(DN_ALPHA * x + g1 * mix, lw['ln_g'][0], lw['ln_b'][0])
    ffn = hier_moe(x * (1.0 + sc2) + sh2, lw)
    x = layer_norm(DN_ALPHA * x + g2 * ffn, lw['ln_g'][1], lw['ln_b'][1])
    return x, ctx_new


def setup_inputs(seed: int = 0) -> dict:
    key = jax.random.key(seed)
    ks = iter(jax.random.split(key, 32))
    f32 = jnp.float32
    nrm = lambda shape, s: jax.random.normal(next(ks), shape, f32) * s
    dsc = D_MODEL ** -0.5
    x_prompt = nrm((BATCH, SEQ, D_MODEL), 1.0)
    x_sample = nrm((DEC_BATCH, DEC_SEQ, D_MODEL), 1.0)
    state_a = nrm((DEC_BATCH, DEPTH, 2, A_HEADS, A_DK, A_DV), 0.1)
    cache_b_kv = nrm((DEC_BATCH, DEPTH, 2, B_HEADS, PAST_LEN, B_V), 1.0)
    cache_c_kv = nrm((DEC_BATCH, DEPTH, 2, C_KV, PAST_LEN, C_DIM), 1.0)
    c = nrm((DEC_BATCH, D_MODEL), 1.0)
    c_ctx = nrm((D_MODEL,), 1.0)
    w_ada = nrm((DEPTH, D_MODEL, 6 * D_MODEL), 0.5 * dsc)
    b_ada = nrm((DEPTH, 6 * D_MODEL), 0.02)
    w_in = nrm((DEPTH, D_MODEL, IN_COLS), dsc)
    a_conv = nrm((DEPTH, CONV_K, 3 * MIX_A), CONV_K ** -0.5)
    a_log = jnp.log(jax.random.uniform(next(ks), (DEPTH, 2, A_HEADS), f32, 1.0, 16.0))
    dt = jnp.exp(jax.random.uniform(next(ks), (DEPTH, 2, A_HEADS), f32, math.log(1e-3), math.log(1e-1)))
    a_dt_bias = dt + jnp.log(-jnp.expm1(-dt))
    a_norm = 1.0 + nrm((DEPTH, A_DV), 0.02)
    b_lambda = nrm((DEPTH, 4, B_QK), 0.1)
    b_norm = 1.0 + nrm((DEPTH, B_V), 0.02)
    c_q_norm = 1.0 + nrm((DEPTH, C_DIM), 0.02)
    c_k_norm = 1.0 + nrm((DEPTH, C_DIM), 0.02)
    w_out = nrm((DEPTH, MIX_WIDTH, D_MODEL), MIX_WIDTH ** -0.5 * DN_BETA)
    ln_g = 1.0 + nrm((DEPTH, 2, D_MODEL), 0.02)
    ln_b = nrm((DEPTH, 2, D_MODEL), 0.02)
    w_group = nrm((DEPTH, D_MODEL, N_GROUPS), dsc)
    b_group = nrm((DEPTH, N_GROUPS), 0.01)
    w_router = nrm((DEPTH, D_MODEL, N_EXPERTS), dsc)
    b_router = nrm((DEPTH, N_EXPERTS), 0.01)
    w_gate = nrm((DEPTH, N_EXPERTS, D_MODEL, EXPERT_FF), dsc)
    w_up = nrm((DEPTH, N_EXPERTS, D_MODEL, EXPERT_FF), dsc)
    w_down = nrm((DEPTH, N_EXPERTS, EXPERT_FF, D_MODEL), EXPERT_FF ** -0.5 * DN_BETA)
    return {'x_prompt': x_prompt, 'x_sample': x_sample, 'state_a': state_a, 'cache_b_kv': cache_b_kv,
            'cache_c_kv': cache_c_kv, 'c': c, 'c_ctx': c_ctx, 'w_ada': w_ada, 'b_ada': b_ada, 'w_in': w_in,
            'a_conv': a_conv, 'a_log': a_log, 'a_dt_bias': a_dt_bias, 'a_norm': a_norm, 'b_lambda': b_lambda,
            'b_norm': b_norm, 'c_q_norm': c_q_norm, 'c_k_norm': c_k_norm, 'w_out': w_out, 'ln_g': ln_g,
            'ln_b': ln_b, 'w_group': w_group, 'b_group': b_group, 'w_router': w_router, 'b_router': b_router,
            'w_gate': w_gate, 'w_up': w_up, 'w_down': w_down}


def reference(x_prompt, x_sample, state_a, cache_b_kv, cache_c_kv, c, c_ctx, w_ada, b_ada, w_in, a_conv,
              a_log, a_dt_bias, a_norm, b_lambda, b_norm, c_q_norm, c_k_norm, w_out, ln_g, ln_b, w_group,
              b_group, w_router, b_router, w_gate, w_up, w_down):
    y_p = x_prompt
    y_s = x_sample
    new_a, new_b, new_c = [], [], []
    for l in range(DEPTH):
        lw = dict(w_ada=w_ada[l], b_ada=b_ada[l], w_in=w_in[l], a_conv=a_conv[l], a_log=a_log[l],
                  a_dt_bias=a_dt_bias[l], a_norm=a_norm[l], b_lambda=b_lambda[l], b_norm=b_norm[l],
                  c_q_norm=c_q_norm[l], c_k_norm=c_k_norm[l], w_out=w_out[l], ln_g=ln_g[l], ln_b=ln_b[l],
                  w_group=w_group[l], b_group=b_group[l], w_router=w_router[l], b_router=b_router[l],
                  w_gate=w_gate[l], w_up=w_up[l], w_down=w_down[l])
        lam_init = 0.8 - 0.6 * math.exp(-0.3 * l)
        y_p, (s_a, kv_b, kv_c) = trunk_layer(y_p, c_ctx[None, :], lw, lam_init, None)
        new_a.append(s_a)
        new_b.append(kv_b)
        new_c.append(kv_c)
        y_s, _ = trunk_layer(y_s, c, lw, lam_init, (state_a[:, l], cache_b_kv[:, l], cache_c_kv[:, l]))
    return (y_p, y_s, jnp.stack(new_a, axis=1), jnp.stack(new_b, axis=1), jnp.stack(new_c, axis=1))
```

```python
import os
import numpy as np
import concourse.bass as bass
import concourse.mybir as mybir
from concourse.bass_utils import run_bass_kernel_spmd

F32 = mybir.dt.float32
BF = mybir.dt.bfloat16
AF = mybir.ActivationFunctionType
ALU = mybir.AluOpType
AX = mybir.AxisListType

D = 2048
KC = 16
T = 2048
L = 2
IN_COLS = 5136
NCORES = 8
ALPHA = (2 * L) ** 0.25
EPS = 1e-6
ESZ = {F32: 4, BF: 2}


class Op:
    __slots__ = ("eng", "fn", "deps", "sig", "sigval", "dma", "slot", "slotval", "n")


class Sched:
    K = 8
    ENGS = ("pe", "act", "dve", "pool", "sp")

    def __init__(self, nc):
        self.nc = nc
        self.ops = {e: [] for e in self.ENGS}
        self.recs = {}
        self.ndma = {e: 0 for e in self.ENGS}
        self.notrack = set()
        self.alldma = []

    @staticmethod
    def _esz(ap):
        return ESZ.get(ap.dtype, 4)

    def box(self, ap):
        es = self._esz(ap)
        dims = list(ap.ap)
        off = ap.offset
        if ap.name in self.dram_names:
            ext = sum(abs(s) * (c - 1) for s, c in dims) + 1
            return (0, 1, off * es, (off + ext) * es)
        pstep, pcount = dims[0]
        if pstep == 0:
            pstep = 1 << 30
        p0 = off // pstep
        f0 = off % pstep
        ext = sum(abs(s) * (c - 1) for s, c in dims[1:]) + 1
        return (p0, p0 + pcount, f0 * es, (f0 + ext) * es)

    @staticmethod
    def _ov(a, b):
        return a[0] < b[1] and b[0] < a[1] and a[2] < b[3] and b[2] < a[3]

    @staticmethod
    def _cover(a, b):
        return a[0] <= b[0] and a[1] >= b[1] and a[2] <= b[2] and a[3] >= b[3]

    def add(self, eng, fn, ins, outs, dma=False):
        op = Op()
        op.eng = eng
        op.fn = fn
        op.sig = False
        op.sigval = 0
        op.dma = dma
        deps = set()
        ins = [a for a in ins if not (a is None or isinstance(a, (int, float)))]
        outs = list(outs) + [a for a in ins if a.name.startswith("ps")]
        ins = [a for a in ins if not a.name.startswith("ps")]
        for ap in ins:
            if ap is None or isinstance(ap, (int, float)) or ap.name in self.notrack:
                continue
            b = self.box(ap)
            for rec in self.recs.get(ap.name, ()):
                if self._ov(rec[0], b):
                    if rec[1] is not None:
                        deps.add(rec[1])
                    rec[2].append(op)
        for ap in outs:
            if ap.name in self.notrack:
                continue
            b = self.box(ap)
            lst = self.recs.setdefault(ap.name, [])
            keep = []
            for rec in lst:
                if self._ov(rec[0], b):
                    if rec[1] is not None:
                        deps.add(rec[1])
                    deps.update(rec[2])
                    if self._cover(b, rec[0]):
                        continue
                keep.append(rec)
            keep.append([b, op, []])
            self.recs[ap.name] = keep
        deps.discard(op)
        if eng == "pe":
            deps = {d for d in deps if not (d.eng == "pe" and not d.dma)}
        for d in deps:
            d.sig = True
        op.deps = deps
        if dma:
            op.n = self.ndma[eng]
            self.ndma[eng] += 1
            op.slot = op.n % self.K
            op.slotval = 16 * (op.n // self.K + 1)
            self.alldma.append(op)
        self.ops[eng].append(op)
        return op

    def barrier(self):
        lasts = []
        for e in self.ENGS:
            for o in reversed(self.ops[e]):
                if not o.dma and o.fn is not None:
                    lasts.append(o)
                    break
        dmas = list(self.alldma)
        self.alldma = []
        for e in self.ENGS:
            op = Op()
            op.eng = e
            op.fn = None
            op.sig = False
            op.sigval = 0
            op.dma = False
            op.deps = set(lasts) | set(dmas)
            if e == "pe":
                op.deps = {d for d in op.deps if not (d.eng == "pe" and not d.dma)}
            for d in op.deps:
                d.sig = True
            self.ops[e].append(op)
        self.recs = {}

    def mm(self, out, lhsT, rhs, start=True, stop=True):
        return self.add("pe", lambda e: e.matmul(out, lhsT, rhs, start=start, stop=stop),
                        [lhsT, rhs], [out])

    def tr(self, out, in_, ident):
        return self.add("pe", lambda e: e.transpose(out, in_, ident), [in_, ident], [out])

    def act(self, out, in_, func, scale=None, bias=None, accum_out=None):
        kw = {}
        ins = [in_]
        if scale is not None:
            kw["scale"] = scale
            if not isinstance(scale, (int, float)):
                ins.append(scale)
        if bias is not None:
            kw["bias"] = bias
            if not isinstance(bias, (int, float)):
                ins.append(bias)
        outs = [out]
        if accum_out is not None:
            kw["accum_out"] = accum_out
            outs.append(accum_out)
        return self.add("act", lambda e: e.activation(out, in_, func, **kw), ins, outs)

    def tt(self, out, a, b, op, eng="dve"):
        return self.add(eng, lambda e: e.tensor_tensor(out, a, b, op), [a, b], [out])

    def ts(self, out, a, s1, op0, s2=None, op1=None, eng="dve"):
        ins = [a] + [x for x in (s1, s2) if x is not None and not isinstance(x, (int, float))]
        if op1 is None:
            return self.add(eng, lambda e: e.tensor_scalar(out, a, s1, None, op0), ins, [out])
        return self.add(eng, lambda e: e.tensor_scalar(out, a, s1, s2, op0, op1), ins, [out])

    def stt(self, out, a, scalar, b, op0, op1):
        ins = [a, b] + ([] if isinstance(scalar, (int, float)) else [scalar])
        return self.add("dve", lambda e: e.scalar_tensor_tensor(out, a, scalar, b, op0, op1),
                        ins, [out])

    def cp(self, out, in_, eng="dve"):
        if eng == "act":
            return self.add("act", lambda e: e.copy(out, in_), [in_], [out])
        return self.add(eng, lambda e: e.tensor_copy(out, in_), [in_], [out])

    def recip(self, out, in_):
        return self.add("dve", lambda e: e.reciprocal(out, in_), [in_], [out])

    def red(self, out, in_, op):
        return self.add("dve", lambda e: e.tensor_reduce(out, in_, AX.X, op), [in_], [out])

    def memset(self, out, val, eng="dve"):
        return self.add(eng, lambda e: e.memset(out, val), [], [out])

    def dma(self, out, in_, q="sp"):
        return self.add(q, lambda e: e.dma_start(out=out, in_=in_), [in_], [out], dma=True)

    def emit(self, sems, dsems):
        nc = self.nc
        for e in self.ENGS:
            c = 0
            for op in self.ops[e]:
                if op.sig and not op.dma:
                    c += 1
                    op.sigval = c
        final = {}
        for e in self.ENGS:
            for op in self.ops[e]:
                if op.dma:
                    final[(e, op.slot)] = max(final.get((e, op.slot), 0), op.slotval)

        def replay(ename, eng):
            seen = {}
            for op in self.ops[ename]:
                need = {}
                for d in op.deps:
                    if d.dma:
                        key = ("d", d.eng, d.slot)
                        val = d.slotval
                    else:
                        key = ("e", d.eng)
                        val = d.sigval
                    if seen.get(key, 0) < val:
                        need[key] = max(need.get(key, 0), val)
                if op.dma and op.n >= self.K:
                    key = ("d", ename, op.slot)
                    val = op.slotval - 16
                    if seen.get(key, 0) < val:
                        need[key] = max(need.get(key, 0), val)
                for key, val in need.items():
                    sem = dsems[key[1]][key[2]] if key[0] == "d" else sems[key[1]]
                    eng.wait_ge(sem, val)
                    seen[key] = val
                if op.fn is None:
                    continue
                ins = op.fn(eng)
                if op.dma:
                    ins.then_inc(dsems[ename][op.slot], 16)
                elif op.sig:
                    ins.then_inc(sems[ename], 1)
            if ename == "sp":
                for (q, slot), val in final.items():
                    if seen.get(("d", q, slot), 0) < val:
                        eng.wait_ge(dsems[q][slot], val)

        with nc.Block() as blk:
            @blk.tensor
            def _(eng):
                replay("pe", eng)

            @blk.scalar
            def _(eng):
                replay("act", eng)

            @blk.vector
            def _(eng):
                replay("dve", eng)

            @blk.gpsimd
            def _(eng):
                replay("pool", eng)

            @blk.sync
            def _(eng):
                replay("sp", eng)


class Arena:
    def __init__(self, t):
        self.t = t
        self.off = 0
        self.n = t.shape[1]

    def mark(self):
        return self.off

    def release(self, m=0):
        self.off = m

    def f32(self, n):
        assert self.off + n <= self.n, ("arena overflow", self.off, n)
        a = self.t[:, self.off:self.off + n]
        self.off += n
        return a

    def bf(self, n):
        m = (n + 1) // 2
        assert self.off + m <= self.n, ("arena overflow", self.off, m)
        a = self.t[:, self.off:self.off + m].bitcast(BF)
        self.off += m
        return a


NCST = 128 + 128 + 4 * 512 + 128 + 128 + 16 * 128
C_ID, C_ONE, C_LE, C_LT, C_GE, C_GT, C_RB, C_RC, C_SEL = (
    0, 128, 256, 768, 1280, 1792, 2304, 2432, 2560)
P_CONV, P_LNG1, P_LNB1, P_LNG2, P_LNB2, P_BADA = 0, 60, 76, 92, 108, 124
P_ANORM, P_BNORM, P_CQN, P_CKN, P_CKNBC = 220, 348, 349, 350, 351
P_ALOG, P_DTB, P_BLAM, P_BGR = 479, 487, 495, 751
PL = 771
P_KEEP = 2 * PL
P_COND = 2 * PL + 1
NPP = 2 * PL + 1 + 16


def _consts():
    c = np.zeros((128, NCST), np.float32)
    a = np.arange(128)
    c[:, C_ID:C_ID + 128] = np.eye(128)
    c[:, C_ONE:C_ONE + 128] = 1.0
    le = (a[:, None] <= a[None, :]).astype(np.float32)
    lt = (a[:, None] < a[None, :]).astype(np.float32)
    ge = (a[:, None] >= a[None, :]).astype(np.float32)
    gt = (a[:, None] > a[None, :]).astype(np.float32)
    for off, m in ((C_LE, le), (C_LT, lt), (C_GE, ge), (C_GT, gt)):
        c[:, off:off + 512] = np.tile(m, (1, 4))
    rb = np.zeros((128, 128), np.float32)
    rc = np.zeros((128, 128), np.float32)
    for d in range(128):
        blk, r = divmod(d, 32)
        rb[d, blk * 32 + (r + 16) % 32] = 1.0
        blk, r = divmod(d, 64)
        rc[d, blk * 64 + (r + 32) % 64] = 1.0
    c[:, C_RB:C_RB + 128] = rb
    c[:, C_RC:C_RC + 128] = rc
    for e in range(16):
        c[e, C_SEL + e * 128:C_SEL + (e + 1) * 128] = 1.0
    return c


def _rope_tables(sample):
    tab = np.zeros((4, 128, T), np.float32)
    tab[0] = 1.0
    tab[2] = 1.0
    if not sample:
        return tab
    t = np.arange(T)
    row = (t // 64).astype(np.float32)
    col = (t % 64).astype(np.float32)

    def fill(ci, si, m):
        fr = (10000.0 ** (-np.arange(0, m, 2, dtype=np.float32) / m)).astype(np.float32)
        half = m // 2
        for d in range(128):
            blk, r = divmod(d, m)
            pos = row if blk % 2 == 0 else col
            f = fr[r % half]
            ang = (pos * f).astype(np.float32)
            tab[ci, d] = np.cos(ang)
            tab[si, d] = np.sin(ang) * (-1.0 if r < half else 1.0)

    fill(0, 1, 32)
    fill(2, 3, 64)
    return tab


def _mask(sample):
    m = np.zeros((128, 8, 20), np.float32)
    if not sample:
        m[:] = -30000.0
        for qs in range(8):
            m[:, qs, 4 + 2 * qs:4 + 2 * qs + 2] = 0.0
    return m.reshape(128, 160)


def _params(inp, cond, sample):
    p = np.zeros((128, NPP), np.float32)
    for l in range(L):
        o = l * PL
        p[:, o + P_CONV:o + P_CONV + 60] = (
            inp["a_conv"][l].reshape(5, 12, 128).transpose(2, 1, 0).reshape(128, 60))
        for k, off in ((0, P_LNG1), (1, P_LNG2)):
            p[:, o + off:o + off + 16] = inp["ln_g"][l, k].reshape(16, 128).T
        for k, off in ((0, P_LNB1), (1, P_LNB2)):
            p[:, o + off:o + off + 16] = inp["ln_b"][l, k].reshape(16, 128).T
        p[:, o + P_BADA:o + P_BADA + 96] = inp["b_ada"][l].reshape(96, 128).T
        p[:, o + P_ANORM:o + P_ANORM + 128] = inp["a_norm"][l][None, :]
        p[:, o + P_BNORM] = inp["b_norm"][l]
        p[:, o + P_CQN] = inp["c_q_norm"][l]
        p[:, o + P_CKN] = inp["c_k_norm"][l]
        p[:, o + P_CKNBC:o + P_CKNBC + 128] = inp["c_k_norm"][l][None, :]
        p[:, o + P_ALOG:o + P_ALOG + 8] = inp["a_log"][l].reshape(1, 8)
        p[:, o + P_DTB:o + P_DTB + 8] = inp["a_dt_bias"][l].reshape(1, 8)
        p[:, o + P_BLAM:o + P_BLAM + 256] = inp["b_lambda"][l].reshape(1, 256)
        p[:, o + P_BGR:o + P_BGR + 4] = inp["b_group"][l][None, :]
        p[:, o + P_BGR + 4:o + P_BGR + 20] = inp["b_router"][l][None, :]
    p[:, P_KEEP] = 1.0 if sample else 0.0
    p[:, P_COND:P_COND + 16] = cond.reshape(16, 128).T
    return p


def build_program(stages="all"):
    nc = bass.Bass("TRN2", target_bir_lowering=False)
    S = Sched(nc)

    def din(name, shape, dt=F32):
        S.notrack.add(name)
        return nc.dram_tensor(name, list(shape), dt, kind="ExternalInput").ap()

    def dout(name, shape):
        return nc.dram_tensor(name, list(shape), F32, kind="ExternalOutput").ap()

    def dscr(name, shape, dt=F32):
        return nc.dram_tensor(name, list(shape), dt, kind="Internal").ap()

    x_in = din("x", [T, D])
    cst_d = din("cst", [128, NCST])
    pp_d = din("pp", [128, NPP])
    mask_d = din("maskb", [128, 160])
    rope_d = din("rope", [4, 128, T])
    s0_d = din("s0", [L, 2, 8, 4, 128, 128])
    cb_d = din("cache_b", [L, 2, 4, 512, 128])
    cc_d = din("cache_c", [L, 2, 2, 512, 128])
    w_ada = din("w_ada", [L, D, 6 * D])
    w_in = din("w_in", [L, D, IN_COLS])
    w_out = din("w_out", [L, D, D])
    wrg_d = din("wrg", [L, 128, 16, 20])
    if stages == "all":
        w_gate = din("w_gate", [L, 16, D, 1024])
        w_up = din("w_up", [L, 16, D, 1024])
        w_down = din("w_down", [L, 16, 1024, D])

    y_out = dout("y", [T, D])
    na_out = dout("new_a", [8, L, 2, 4, 128, 128])
    nb_out = dout("new_b", [8, L, 2, 4, 256, 128])
    ncc_out = dout("new_c", [8, L, 2, 2, 256, 128])

    XT = dscr("XT", [16, 128, T])
    AQ = dscr("AQ", [12, 128, T])
    AQ2 = dscr("AQ2", [12, 128, T])
    AG = dscr("AG", [4, 128, T])
    OTd = dscr("OTd", [16, 128, T], BF)
    QBd = dscr("QBd", [4, 128, T], BF)
    KBd = dscr("KBd", [4, 128, 2560], BF)
    VBd = dscr("VBd", [20, 128, 4, 128], BF)
    QCd = dscr("QCd", [8, 128, T], BF)
    KCd = dscr("KCd", [2, 128, 2560], BF)
    VCd = dscr("VCd", [20, 128, 2, 128], BF)
    H2d = dscr("H2d", [16, 128, T], BF)
    GBd = dscr("GBd", [16, 128, T])
    S.dram_names = {"XT", "AQ", "AQ2", "AG", "OTd", "QBd", "KBd", "VBd", "QCd", "KCd", "VCd",
                    "H2d", "GBd", "y", "new_a", "new_b", "new_c"}

    ARN = 45056
    import contextlib
    with contextlib.ExitStack() as st:
        ar_t = st.enter_context(nc.sbuf_tensor("arena", [128, ARN], F32))
        cst = st.enter_context(nc.sbuf_tensor("cst_s", [128, NCST], F32))
        pp = st.enter_context(nc.sbuf_tensor("pp_s", [128, NPP], F32))
        mask = st.enter_context(nc.sbuf_tensor("mask_s", [128, 160], F32))
        md = st.enter_context(nc.sbuf_tensor("md_s", [128, L * 96], F32))
        misc = st.enter_context(nc.sbuf_tensor("misc_s", [128, 512], F32))
        onesb_t = st.enter_context(nc.sbuf_tensor("onesb_s", [128, 128], BF))
        bat_t = st.enter_context(nc.sbuf_tensor("bat_s", [128, 256], F32))
        PS = [st.enter_context(nc.psum_tensor(f"ps{i}", [128, 512], F32)) for i in range(8)]
        sems = {e: st.enter_context(nc.semaphore(f"sem_{e}")) for e in ("pe", "act", "dve", "pool")}
        dsems = {q: [st.enter_context(nc.semaphore(f"dsem_{q}{i}")) for i in range(Sched.K)]
                 for q in ("sp", "act", "pool")}

        A = Arena(ar_t)
        ident = cst[:, C_ID:C_ID + 128]
        ones = cst[:, C_ONE:C_ONE + 128]
        onesb = onesb_t[:, :]

        S.dma(cst[:, :], cst_d)
        S.dma(pp[:, :], pp_d)
        S.dma(mask[:, :], mask_d)
        S.cp(onesb, ones)
        scond = misc[:, 0:16]
        S.act(scond, pp[:, P_COND:P_COND + 16], AF.Silu)

        def mdv(l, j):
            return md[:, l * 96 + j * 16:l * 96 + (j + 1) * 16]

        if True:
            m0 = A.mark()
            modrow = A.f32(6 * D)
            wts = [A.f32(16 * 512).rearrange("p (k n) -> p k n", k=16) for _ in range(2)]
            for l in range(L):
                for ct in range(24):
                    wt = wts[ct % 2]
                    S.dma(wt, w_ada[l, :, ct * 512:(ct + 1) * 512].rearrange("(k p) n -> p k n", p=128))
                    pb = PS[ct % 2]
                    for kc in range(KC):
                        S.mm(pb[0:1, :], scond[:, kc:kc + 1], wt[:, kc, :], start=(kc == 0), stop=(kc == KC - 1))
                    S.cp(modrow[0:1, ct * 512:(ct + 1) * 512], pb[0:1, :], eng="act")
                pm = PS[2]
                for q in range(96):
                    S.mm(pm[:, q:q + 1], modrow[0:1, q * 128:(q + 1) * 128], ones[0:1, 0:1])
                o = l * PL
                S.tt(md[:, l * 96:(l + 1) * 96], pm[:, 0:96], pp[:, o + P_BADA:o + P_BADA + 96], ALU.add)
                S.ts(mdv(l, 1), mdv(l, 1), 1.0, ALU.add)
                S.ts(mdv(l, 4), mdv(l, 4), 1.0, ALU.add)
                S.ts(mdv(l, 2), mdv(l, 2), 1.0 / ALPHA, ALU.mult)
                S.ts(mdv(l, 5), mdv(l, 5), 1.0 / ALPHA, ALU.mult)
            S.barrier()
            A.release(m0)

        xts = [A.f32(D) for _ in range(2)]
        stg = [A.f32(D).rearrange("p (k t) -> p k t", k=16) for _ in range(2)]
        for tb in range(16):
            xt = xts[tb % 2]
            S.dma(xt, x_in[tb * 128:(tb + 1) * 128, :])
            sg = stg[tb % 2]
            for k4 in range(4):
                pb = PS[(tb * 4 + k4) % 4]
                for j in range(4):
                    kc = k4 * 4 + j
                    S.tr(pb[:, j * 128:(j + 1) * 128], xt[:, kc * 128:(kc + 1) * 128], ident)
                S.cp(sg[:, k4 * 4:(k4 + 1) * 4, :], pb[:, :].rearrange("p (k t) -> p k t", k=4),
                     eng=("act" if k4 % 2 else "dve"))
            S.dma(XT[:, :, tb * 128:(tb + 1) * 128].rearrange("k p t -> p k t"), sg)
        S.barrier()
        A.release(0)

        GCOLS = [0, 512, 1024, 1536, 2064, 2576, 3088, 3600, 4112, 4624]

        def phase_proj(l, upto=9):
            o = l * PL
            sh1, sc1p = mdv(l, 0), mdv(l, 1)
            hb = A.bf(16 * T).rearrange("p (k t) -> p k t", k=16)
            BAt = bat_t[:, :].rearrange("p (b c) -> p b c", b=16)
            wba = A.f32(256).rearrange("p (k c) -> p k c", k=16)
            baT = A.f32(512)
            S.dma(wba, w_in[l, :, 2048:2064].rearrange("(k p) n -> p k n", p=128))
            m2 = A.mark()
            xcs = [A.f32(512) for _ in range(3)]
            hcs = [A.f32(512) for _ in range(2)]
            for tt in range(4):
                tsl = slice(tt * 512, (tt + 1) * 512)
                pba = PS[4 + tt % 2]
                for kc in range(KC):
                    xc = xcs[(tt * 16 + kc) % 3]
                    S.dma(xc, XT[kc, :, tsl])
                    hc = hcs[kc % 2]
                    S.act(hc, xc, AF.Identity, scale=sc1p[:, kc:kc + 1], bias=sh1[:, kc:kc + 1])
                    S.cp(hb[:, kc, tsl], hc)
                    S.mm(pba[0:16, :], wba[:, kc, :], hc, start=(kc == 0), stop=(kc == KC - 1))
                S.cp(baT[0:16, :], pba[0:16, :], eng="act")
                pt = PS[6]
                for blk in range(4):
                    S.tr(pt[:, blk * 16:(blk + 1) * 16], baT[0:16, blk * 128:(blk + 1) * 128], ident[0:16, 0:16])
                S.cp(BAt[:, tt * 4:(tt + 1) * 4, :], pt[:, 0:64].rearrange("p (b c) -> p b c", b=4))
            S.barrier()
            A.release(m2)
            if upto == 1:
                A.release(0)
                return
            ck = [A.f32(512).rearrange("p (b d) -> p b d", b=4) for _ in range(2)]
            kb16 = [A.bf(512) for _ in range(2)]
            for i, (cd, nh, Kd) in enumerate(((cb_d, 4, KBd), (cc_d, 2, KCd))):
                for h in range(nh):
                    c_ = ck[h % 2]
                    S.dma(c_, cd[l, 0, h].rearrange("(b p) d -> p b d", p=128))
                    pb = PS[h % 2]
                    for b in range(4):
                        S.tr(pb[:, b * 128:(b + 1) * 128], c_[:, b, :], ident)
                    k16 = kb16[h % 2]
                    S.cp(k16, pb[:, :])
                    S.dma(Kd[h, :, 0:512], k16)
            cv = A.f32(2048).rearrange("p (b h d) -> p b h d", b=4, h=4)
            cv16 = A.bf(2048).rearrange("p (b h d) -> p b h d", b=4, h=4)
            for b in range(4):
                S.dma(cv[:, b], cb_d[l, 1, :, b * 128:(b + 1) * 128, :].rearrange("h p d -> p h d"))
            S.cp(cv16, cv)
            for b in range(4):
                S.dma(VBd[b], cv16[:, b])
            cv2 = A.f32(1024).rearrange("p (b h d) -> p b h d", b=4, h=2)
            cv216 = A.bf(1024).rearrange("p (b h d) -> p b h d", b=4, h=2)
            for b in range(4):
                S.dma(cv2[:, b], cc_d[l, 1, :, b * 128:(b + 1) * 128, :].rearrange("h p d -> p h d"))
            S.cp(cv216, cv2)
            for b in range(4):
                S.dma(VCd[b], cv216[:, b])
            S.barrier()
            A.release(m2)
            if upto == 2:
                A.release(0)
                return
            tabs = A.f32(4 * T).rearrange("p (c t) -> p c t", c=4)
            S.dma(tabs, rope_d.rearrange("c p t -> p c t"))
            wts = [A.bf(16 * 512).rearrange("p (k n) -> p k n", k=16) for _ in range(3)]
            st32 = [A.f32(512) for _ in range(3)]
            u32s = [A.f32(512) for _ in range(2)]
            sqs = [A.f32(512) for _ in range(2)]
            t1s = [A.f32(512) for _ in range(2)]
            obf = [A.bf(512) for _ in range(3)]
            tks = [A.f32(512) for _ in range(2)]
            tk16 = [A.bf(512) for _ in range(2)]
            sm = A.f32(8)
            cnt = 0
            for g in range(10):
                wt = wts[g % 3]
                S.dma(wt, w_in[l, :, GCOLS[g]:GCOLS[g] + 512].rearrange("(k p) n -> p k n", p=128), q="pool")
                for j in range(4):
                    ci = g * 4 + j
                    if g == 6 or (g == 9 and j >= 2):
                        continue
                    if os.environ.get("DBG_NOFM") and ci >= int(os.environ["DBG_NOFM"]):
                        continue
                    for tt in range(4):
                        tsl = slice(tt * 512, (tt + 1) * 512)
                        cnt += 1
                        pb = PS[cnt % 3]
                        for kc in range(KC):
                            S.mm(pb[:, :], wt[:, kc, j * 128:(j + 1) * 128], hb[:, kc, tsl],
                                 start=(kc == 0), stop=(kc == KC - 1))
                        if ci < 12:
                            s_ = st32[cnt % 3]
                            S.cp(s_, pb[:, :], eng="act")
                            S.dma(AQ[ci, :, tsl], s_)
                            continue
                        if ci < 16:
                            s_ = st32[cnt % 3]
                            S.act(s_, pb[:, :], AF.Silu)
                            S.dma(AG[ci - 12, :, tsl], s_)
                            continue
                        isB = ci < 28
                        u = u32s[cnt % 2]
                        S.cp(u, pb[:, :], eng="act")
                        if not isB:
                            sq = sqs[cnt % 2]
                            S.act(sq, pb[:, :], AF.Square)
                            pr = PS[3]
                            S.mm(pr[:, :], ones, sq)
                            rs = t1s[cnt % 2]
                            S.ts(rs, pr[:, :], 1.0 / 128, ALU.mult, EPS, ALU.add)
                            S.act(rs, rs, AF.Sqrt)
                            S.recip(rs, rs)
                            wcol = pp[:, o + P_CQN:o + P_CQN + 1] if ci < 36 else pp[:, o + P_CKN:o + P_CKN + 1]
                            S.stt(u, u, wcol, rs, ALU.mult, ALU.mult)
                        R = cst[:, C_RB:C_RB + 128] if isB else cst[:, C_RC:C_RC + 128]
                        ti = 0 if isB else 2
                        pr2 = PS[4]
                        S.mm(pr2[:, :], R, u)
                        t1 = t1s[(cnt + 1) % 2] if not isB else t1s[cnt % 2]
                        S.tt(t1, pr2[:, :], tabs[:, ti + 1, tsl], ALU.mult)
                        S.tt(u, u, tabs[:, ti, tsl], ALU.mult)
                        ob = obf[cnt % 3]
                        S.tt(ob, u, t1, ALU.add)
                        if ci < 20:
                            dst = QBd[ci - 16, :, tsl]
                        elif ci < 24:
                            dst = KBd[ci - 20, :, 512 + tt * 512:512 + (tt + 1) * 512]
                        elif ci < 36:
                            dst = QCd[ci - 28, :, tsl]
                        else:
                            dst = KCd[ci - 36, :, 512 + tt * 512:512 + (tt + 1) * 512]
                        S.dma(dst, ob)
                if g in (5, 6, 9) and not os.environ.get("DBG_NOTM") and str(g) in os.environ.get("DBG_TMG", "569"):
                    for tb in range(16):
                        pb = PS[5 + tb % 2]
                        for kc in range(KC):
                            S.mm(pb[:, :], hb[:, kc, tb * 128:(tb + 1) * 128], wt[:, kc, :],
                                 start=(kc == 0), stop=(kc == KC - 1))
                        seg, tl = tb // 2, (tb % 2) * 128
                        s_ = tks[tb % 2]
                        if g in (5, 6):
                            S.cp(s_, pb[:, :], eng="act")
                            S.dma(nb_out[seg, l, g - 5, :, tl:tl + 128, :].rearrange("h t d -> t h d"),
                                  s_.rearrange("p (h d) -> p h d", h=4))
                            if g == 6:
                                v16 = tk16[tb % 2]
                                S.cp(v16, pb[:, :])
                                S.dma(VBd[4 + tb], v16.rearrange("p (h d) -> p h d", h=4))
                        else:
                            for g2 in range(2):
                                S.act(sqs[g2][:, 0:128], pb[:, g2 * 128:(g2 + 1) * 128], AF.Square,
                                      accum_out=sm[:, g2:g2 + 1])
                            S.ts(sm[:, 2:4], sm[:, 0:2], 1.0 / 128, ALU.mult, EPS, ALU.add)
                            S.act(sm[:, 2:4], sm[:, 2:4], AF.Sqrt)
                            S.recip(sm[:, 4:6], sm[:, 2:4])
                            for g2 in range(2):
                                S.stt(s_[:, g2 * 128:(g2 + 1) * 128], pb[:, g2 * 128:(g2 + 1) * 128],
                                      sm[:, 4 + g2:5 + g2], pp[:, o + P_CKNBC:o + P_CKNBC + 128],
                                      ALU.mult, ALU.mult)
                            S.cp(s_[:, 256:512], pb[:, 256:512], eng="act")
                            S.dma(ncc_out[seg, l, 0, :, tl:tl + 128, :].rearrange("h t d -> t h d"),
                                  s_[:, 0:256].rearrange("p (h d) -> p h d", h=2))
                            S.dma(ncc_out[seg, l, 1, :, tl:tl + 128, :].rearrange("h t d -> t h d"),
                                  s_[:, 256:512].rearrange("p (h d) -> p h d", h=2))
                            v16 = tk16[tb % 2]
                            S.cp(v16[:, 0:256], pb[:, 256:512])
                            S.dma(VCd[4 + tb], v16[:, 0:256].rearrange("p (h d) -> p h d", h=2))
            S.barrier()
            A.release(0)
            return BAt

        def phase_delta(l):
            o = l * PL
            BAt = bat_t[:, :].rearrange("p (b c) -> p b c", b=16)
            keep = pp[:, P_KEEP:P_KEEP + 1]
            pad = A.f32(8 * 260).rearrange("p (s c) -> p s c", s=8)
            cv = A.f32(T)
            cv3 = cv.rearrange("p (s c) -> p s c", s=8)
            sqb = A.f32(T)
            rsb = A.f32(512)
            for ci in range(12):
                S.dma(pad[:, :, 2:258], AQ[ci].rearrange("p (s c) -> p s c", s=8))
                S.memset(pad[:, 0:1, 0:2], 0.0)
                S.memset(pad[:, 7:8, 258:260], 0.0)
                S.ts(pad[:, 1:8, 0:2], pad[:, 0:7, 256:258], keep, ALU.mult)
                S.ts(pad[:, 0:7, 258:260], pad[:, 1:8, 2:4], keep, ALU.mult)

                def w(j):
                    c0 = o + P_CONV + ci * 5 + j
                    return pp[:, c0:c0 + 1]
                S.ts(cv3, pad[:, :, 0:256], w(0), ALU.mult)
                for j in range(1, 5):
                    S.stt(cv3, pad[:, :, j:j + 256], w(j), cv3, ALU.mult, ALU.add)
                S.act(cv, cv, AF.Silu)
                if ci < 8:
                    S.act(sqb, cv, AF.Square)
                    for tt in range(4):
                        tsl = slice(tt * 512, (tt + 1) * 512)
                        pr = PS[tt % 2]
                        S.mm(pr[:, :], ones, sqb[:, tsl])
                        S.ts(rsb, pr[:, :], EPS, ALU.add)
                        S.act(rsb, rsb, AF.Sqrt)
                        S.recip(rsb, rsb)
                        if ci < 4:
                            S.stt(cv[:, tsl], cv[:, tsl], 128.0 ** -0.5, rsb, ALU.mult, ALU.mult)
                        else:
                            S.tt(cv[:, tsl], cv[:, tsl], rsb, ALU.mult)
                S.dma(AQ2[ci], cv)
            S.barrier()
            A.release(0)
            Bt = A.f32(128).rearrange("p (b c) -> p b c", b=16)
            Gt = A.f32(128).rearrange("p (b c) -> p b c", b=16)
            nA = A.f32(8)
            S.act(nA, pp[:, o + P_ALOG:o + P_ALOG + 8], AF.Exp)
            S.ts(nA, nA, -1.0, ALU.mult)
            S.act(Bt, BAt[:, :, 0:8], AF.Sigmoid)
            S.tt(Gt, BAt[:, :, 8:16], pp[:, o + P_DTB:o + P_DTB + 8].unsqueeze(1).to_broadcast([128, 16, 8]), ALU.add)
            S.act(Gt, Gt, AF.Exp)
            S.act(Gt, Gt, AF.Ln, bias=1.0)
            S.tt(Gt, Gt, nA.unsqueeze(1).to_broadcast([128, 16, 8]), ALU.mult)
            def B512():
                return A.f32(512)

            def v3(a):
                return a.rearrange("p (h j) -> p h j", h=4)

            def hs(a, h):
                return a[:, h * 128:(h + 1) * 128]
            qkv = A.f32(12 * 128).rearrange("p (c t) -> p c t", c=12)
            gm, Dsb, E, t1, qk, N, NT, qkT, RT = (B512() for _ in range(9))
            Pa, PTa, Pb, PTb = (B512() for _ in range(4))
            vb, kdec, rhs2, dl, qss, s0t, Irep = (B512() for _ in range(7))
            Sst = [B512() for _ in range(2)]
            otok = A.f32(16 * 512).rearrange("p (b x) -> p b x", b=16)
            sc12 = A.f32(12)
            nb = A.f32(4)
            bx = A.f32(4)
            S.tt(Irep, cst[:, C_LE:C_LE + 512], cst[:, C_GE:C_GE + 512], ALU.mult)
            for dr in range(2):
                Uin, Lst, Linc, Lstr = (C_LE, C_GT, C_GE, C_GT) if dr == 0 else (C_GE, C_LT, C_LE, C_LT)
                St = Sst[dr]
                S.memset(St, 0.0)
                order = range(16) if dr == 0 else range(15, -1, -1)
                for tb in order:
                    seg = tb // 2
                    is_start = (tb % 2 == 0) if dr == 0 else (tb % 2 == 1)
                    g4 = Gt[:, tb, dr * 4:dr * 4 + 4]
                    b4 = Bt[:, tb, dr * 4:dr * 4 + 4]

                    def bc(a):
                        return a.unsqueeze(2).to_broadcast([128, 4, 128])
                    S.dma(qkv, AQ2[:, :, tb * 128:(tb + 1) * 128].rearrange("c p t -> p c t"))
                    if is_start:
                        S.dma(v3(s0t), s0_d[l, dr, seg].rearrange("h k v -> k h v"))
                        S.stt(St, St, keep, s0t, ALU.mult, ALU.add)
                    S.tt(v3(gm), v3(cst[:, Lst:Lst + 512]), bc(g4), ALU.mult)
                    S.mm(PS[0][:, :], cst[:, Uin:Uin + 128], gm)
                    S.mm(PS[1][:, 0:4], cst[:, Uin:Uin + 128], g4)
                    S.mm(PS[1][:, 4:8], cst[:, Lst:Lst + 128], g4)
                    S.mm(PS[1][:, 8:12], ones, g4)
                    S.cp(Dsb, PS[0][:, :])
                    S.act(E, Dsb, AF.Exp)
                    S.cp(sc12, PS[1][:, 0:12])
                    S.act(sc12, sc12, AF.Exp)
                    eg, er, ge = sc12[:, 0:4], sc12[:, 4:8], sc12[:, 8:12]
                    for h in range(4):
                        S.mm(hs(PS[2], h), qkv[:, h, :], qkv[:, 4 + h, :])
                        S.mm(hs(PS[3], h), qkv[:, 4 + h, :], qkv[:, 4 + h, :])
                    for h in range(4):
                        S.tr(hs(PS[4], h), qkv[:, 4 + h, :], ident)
                        S.tr(hs(PS[5], h), qkv[:, 8 + h, :], ident)
                    S.tt(t1, E, cst[:, Linc:Linc + 512], ALU.mult)
                    S.tt(qk, PS[2][:, :], t1, ALU.mult)
                    S.tt(t1, E, cst[:, Lstr:Lstr + 512], ALU.mult)
                    S.tt(N, PS[3][:, :], t1, ALU.mult)
                    S.ts(nb, b4, -1.0, ALU.mult)
                    S.tt(v3(N), v3(N), bc(nb), ALU.mult)
                    S.tt(bx, b4, eg, ALU.mult)
                    S.tt(v3(kdec), v3(PS[4][:, :]), bc(er), ALU.mult)
                    S.tt(v3(vb), v3(PS[5][:, :]), bc(b4), ALU.mult)
                    for h in range(4):
                        S.tr(hs(PS[6], h), hs(N, h), ident)
                        S.tr(hs(PS[7], h), hs(qk, h), ident)
                    S.cp(NT, PS[6][:, :])
                    S.cp(qkT, PS[7][:, :])
                    S.tt(RT, NT, Irep, ALU.add)
                    P_, PT_ = N, NT
                    for stg in range(6):
                        Pn, PTn = (Pa, PTa) if stg % 2 == 0 else (Pb, PTb)
                        for h in range(4):
                            S.mm(hs(PS[0], h), hs(PT_, h), hs(P_, h))
                        if stg < 5:
                            for h in range(4):
                                S.mm(hs(PS[1], h), hs(P_, h), hs(PT_, h))
                        S.cp(Pn, PS[0][:, :])
                        if stg < 5:
                            S.cp(PTn, PS[1][:, :])
                        for h in range(4):
                            S.mm(hs(PS[2], h), hs(Pn, h), hs(RT, h))
                        S.tt(RT, RT, PS[2][:, :], ALU.add)
                        P_, PT_ = Pn, PTn
                    for h in range(4):
                        S.mm(hs(PS[3], h), qkv[:, 4 + h, :], hs(St, h))
                        S.mm(hs(PS[4], h), qkv[:, h, :], hs(St, h))
                    S.tt(v3(t1), v3(PS[3][:, :]), bc(bx), ALU.mult)
                    S.tt(rhs2, vb, t1, ALU.subtract)
                    S.tt(v3(qss), v3(PS[4][:, :]), bc(eg), ALU.mult)
                    for h in range(4):
                        S.mm(hs(PS[5], h), hs(RT, h), hs(rhs2, h))
                    S.cp(dl, PS[5][:, :])
                    for h in range(4):
                        S.mm(hs(PS[6], h), hs(qkT, h), hs(dl, h))
                        S.mm(hs(PS[7], h), hs(kdec, h), hs(dl, h))
                    if dr == 0:
                        S.tt(otok[:, tb, :], PS[6][:, :], qss, ALU.add)
                    else:
                        S.tt(t1, PS[6][:, :], qss, ALU.add)
                        S.tt(otok[:, tb, :], otok[:, tb, :], t1, ALU.add)
                    for h in range(4):
                        S.stt(hs(St, h), hs(St, h), ge[:, h:h + 1], hs(PS[7], h), ALU.mult, ALU.add)
                    if not is_start:
                        S.dma(na_out[seg, l, dr].rearrange("h k v -> k h v"), v3(St))
                    S.barrier()
            ss = A.f32(8)
            junk = A.f32(128)
            on = A.f32(512)
            ag = [A.f32(512) for _ in range(2)]
            ob4 = [A.bf(512) for _ in range(2)]
            for tb in range(16):
                ot = otok[:, tb, :]
                for h in range(4):
                    S.act(junk, hs(ot, h), AF.Square, accum_out=ss[:, h:h + 1])
                S.ts(ss[:, 4:8], ss[:, 0:4], 1.0 / 128, ALU.mult, EPS, ALU.add)
                S.act(ss[:, 4:8], ss[:, 4:8], AF.Sqrt)
                S.recip(ss[:, 4:8], ss[:, 4:8])
                for h in range(4):
                    S.stt(hs(on, h), hs(ot, h), ss[:, 4 + h:5 + h], pp[:, o + P_ANORM:o + P_ANORM + 128],
                          ALU.mult, ALU.mult)
                pb = PS[tb % 2]
                for h in range(4):
                    S.tr(hs(pb, h), hs(on, h), ident)
                a_ = ag[tb % 2]
                S.dma(v3(a_), AG[:, :, tb * 128:(tb + 1) * 128].rearrange("c p t -> p c t"))
                ob = ob4[tb % 2]
                S.tt(ob, pb[:, :], a_, ALU.mult)
                S.dma(OTd[0:4, :, tb * 128:(tb + 1) * 128].rearrange("c p t -> p c t"), v3(ob))
            S.barrier()
            A.release(0)

        def layer_norm_fm(xt, gcol, bcol, bufs):
            sqs_, mean, msq, var = bufs
            pm, pq = PS[2], PS[3]
            for kc in range(KC):
                sqc = sqs_[kc % 2]
                S.act(sqc, xt[:, kc, :], AF.Square)
                S.mm(pm[:, :], ones, xt[:, kc, :], start=(kc == 0), stop=(kc == KC - 1))
                S.mm(pq[:, :], ones, sqc, start=(kc == 0), stop=(kc == KC - 1))
            S.ts(mean, pm[:, :], 1.0 / D, ALU.mult)
            S.tt(msq, mean, mean, ALU.mult)
            S.stt(var, pq[:, :], 1.0 / D, msq, ALU.mult, ALU.subtract)
            S.ts(var, var, EPS / (ALPHA * ALPHA), ALU.add)
            S.act(var, var, AF.Sqrt)
            S.recip(var, var)
            S.tt(xt, xt, mean.unsqueeze(1).to_broadcast([128, 16, 512]), ALU.subtract)
            S.tt(xt, xt, var.unsqueeze(1).to_broadcast([128, 16, 512]), ALU.mult)
            for kc in range(KC):
                S.act(xt[:, kc, :], xt[:, kc, :], AF.Identity, scale=gcol[:, kc:kc + 1], bias=bcol[:, kc:kc + 1])

        def phase_mix_out(l):
            o = l * PL
            ga1, sh2, sc2p = mdv(l, 2), mdv(l, 3), mdv(l, 4)
            lng = pp[:, o + P_LNG1:o + P_LNG1 + 16]
            lnb = pp[:, o + P_LNB1:o + P_LNB1 + 16]
            wo = A.bf(16 * D).rearrange("p (k n) -> p k n", k=16)
            for c4 in range(4):
                S.dma(wo[:, :, c4 * 512:(c4 + 1) * 512],
                      w_out[l, :, c4 * 512:(c4 + 1) * 512].rearrange("(k p) n -> p k n", p=128), q="pool")
            wrg = A.f32(320).rearrange("p (k c) -> p k c", k=16)
            S.dma(wrg, wrg_d[l])
            lg = A.f32(320).rearrange("p (b c) -> p b c", b=16)
            lgT = A.f32(512)
            ots = [A.bf(16 * 512).rearrange("p (k t) -> p k t", k=16) for _ in range(1)]
            xt = A.f32(16 * 512).rearrange("p (k t) -> p k t", k=16)
            h2b = A.bf(16 * 512).rearrange("p (k t) -> p k t", k=16)
            h2c = [A.f32(512) for _ in range(2)]
            lnb_ = ([A.f32(512) for _ in range(2)], A.f32(512), A.f32(512), A.f32(512))
            for tt in range(4):
                tsl = slice(tt * 512, (tt + 1) * 512)
                ot = ots[0]
                S.dma(ot, OTd[:, :, tsl].rearrange("c p t -> p c t"))
                S.dma(xt, XT[:, :, tsl].rearrange("c p t -> p c t"))
                for oc in range(KC):
                    pb = PS[oc % 2]
                    for kc in range(KC):
                        S.mm(pb[:, :], wo[:, kc, oc * 128:(oc + 1) * 128], ot[:, kc, :],
                             start=(kc == 0), stop=(kc == KC - 1))
                    S.stt(xt[:, oc, :], pb[:, :], ga1[:, oc:oc + 1], xt[:, oc, :], ALU.mult, ALU.add)
                layer_norm_fm(xt, lng, lnb, lnb_)
                S.dma(XT[:, :, tsl].rearrange("c p t -> p c t"), xt)
                pr = PS[4]
                for kc in range(KC):
                    hc = h2c[kc % 2]
                    S.act(hc, xt[:, kc, :], AF.Identity, scale=sc2p[:, kc:kc + 1], bias=sh2[:, kc:kc + 1])
                    S.cp(h2b[:, kc, :], hc)
                    S.mm(pr[0:20, :], wrg[:, kc, :], hc, start=(kc == 0), stop=(kc == KC - 1))
                S.dma(H2d[:, :, tsl].rearrange("c p t -> p c t"), h2b)
                S.cp(lgT[0:20, :], pr[0:20, :])
                pt = PS[5]
                for blk in range(4):
                    S.tr(pt[:, blk * 20:(blk + 1) * 20], lgT[0:20, blk * 128:(blk + 1) * 128], ident[0:20, 0:20])
                S.tt(lg[:, tt * 4:(tt + 1) * 4, :], pt[:, 0:80].rearrange("p (b c) -> p b c", b=4),
                     pp[:, o + P_BGR:o + P_BGR + 20].unsqueeze(1).to_broadcast([128, 4, 20]), ALU.add)
            def sm(n):
                return A.f32(n)
            gl = lg[:, :, 0:4]
            el4 = lg[:, :, 4:20].rearrange("p b (g e) -> p b g e", g=4)
            gmax, gsum, gv, m1, m2, d21, w1, w2 = (sm(16) for _ in range(8))
            ohg, gex, sel, oh1, msk, oh2, ge4, tmp4 = (sm(64).rearrange("p (b c) -> p b c", b=16) for _ in range(8))
            prod = sm(256).rearrange("p (b g e) -> p b g e", b=16, g=4)
            gates = sm(256)
            gates4 = gates.rearrange("p (b g e) -> p b g e", b=16, g=4)

            def b3(a):
                return a.unsqueeze(2).to_broadcast([128, 16, 4])
            S.red(gmax, gl, ALU.max)
            S.tt(ohg, gl, b3(gmax), ALU.is_equal)
            S.tt(gex, gl, b3(gmax), ALU.subtract)
            S.act(gex, gex, AF.Exp)
            S.red(gsum, gex, ALU.add)
            S.recip(gv, gsum)
            S.tt(prod, el4, ohg.unsqueeze(3).to_broadcast([128, 16, 4, 4]), ALU.mult)
            S.red(sel, prod.rearrange("p b g e -> p b e g"), ALU.add)
            S.red(m1, sel, ALU.max)
            S.tt(oh1, sel, b3(m1), ALU.is_equal)
            S.stt(msk, oh1, -1.0e30, sel, ALU.mult, ALU.add)
            S.red(m2, msk, ALU.max)
            S.tt(oh2, msk, b3(m2), ALU.is_equal)
            S.tt(d21, m2, m1, ALU.subtract)
            S.act(d21, d21, AF.Exp)
            S.ts(w1, d21, 1.0, ALU.add)
            S.recip(w1, w1)
            S.tt(w2, d21, w1, ALU.mult)
            S.tt(w1, w1, gv, ALU.mult)
            S.tt(w2, w2, gv, ALU.mult)
            S.tt(ge4, oh1, b3(w1), ALU.mult)
            S.tt(tmp4, oh2, b3(w2), ALU.mult)
            S.tt(ge4, ge4, tmp4, ALU.add)
            S.tt(gates4, ohg.unsqueeze(3).to_broadcast([128, 16, 4, 4]),
                 ge4.unsqueeze(2).to_broadcast([128, 16, 4, 4]), ALU.mult)
            gT = A.f32(T)
            for tb in range(16):
                pb = PS[6 + (tb // 4) % 2]
                S.tr(pb[0:16, (tb % 4) * 128:(tb % 4 + 1) * 128], gates[:, tb * 16:(tb + 1) * 16], ident)
                if tb % 4 == 3:
                    S.cp(gT[0:16, (tb // 4) * 512:(tb // 4 + 1) * 512], pb[0:16, :])
            gbs = [A.f32(512) for _ in range(3)]
            for e in range(16):
                for tt in range(4):
                    tsl = slice(tt * 512, (tt + 1) * 512)
                    pb = PS[(e * 4 + tt) % 2]
                    S.mm(pb[:, :], cst[0:16, C_SEL + e * 128:C_SEL + (e + 1) * 128], gT[0:16, tsl])
                    gb = gbs[(e * 4 + tt) % 3]
                    S.cp(gb, pb[:, :])
                    S.dma(GBd[e, :, tsl], gb)
            S.barrier()
            A.release(0)

        def phase_moe(l):
            o = l * PL
            ga2 = mdv(l, 5)
            lng = pp[:, o + P_LNG2:o + P_LNG2 + 16]
            lnb = pp[:, o + P_LNB2:o + P_LNB2 + 16]
            h2b = A.bf(16 * 512).rearrange("p (k t) -> p k t", k=16)
            yacc = A.f32(16 * 512).rearrange("p (k t) -> p k t", k=16)
            wgs = [A.bf(16 * 512).rearrange("p (k f) -> p k f", k=16) for _ in range(2)]
            wus = [A.bf(16 * 512).rearrange("p (k f) -> p k f", k=16) for _ in range(2)]
            wds = [A.bf(4 * D).rearrange("p (c d) -> p c d", c=4) for _ in range(2)]
            gbs = [A.f32(512) for _ in range(2)]
            sgs = [A.f32(512) for _ in range(2)]
            tms = [A.f32(512) for _ in range(2)]
            actb = [[A.bf(512) for _ in range(4)] for _ in range(2)]
            xcs = [A.f32(512) for _ in range(2)]
            lnb_ = (sgs, tms[0], tms[1], gbs[0])
            ui = 0
            for tm in range(4):
                tsl = slice(tm * 512, (tm + 1) * 512)
                S.dma(h2b, H2d[:, :, tsl].rearrange("c p t -> p c t"))
                for e in range(16):
                    gb = gbs[e % 2]
                    S.dma(gb, GBd[e, :, tsl])
                    for hf in range(2):
                        wg, wu, wd = wgs[ui % 2], wus[ui % 2], wds[ui % 2]
                        ab = actb[ui % 2]
                        ui += 1
                        fsl = slice(hf * 512, (hf + 1) * 512)
                        S.dma(wg, w_gate[l, e, :, fsl].rearrange("(k p) f -> p k f", p=128), q="pool")
                        S.dma(wu, w_up[l, e, :, fsl].rearrange("(k p) f -> p k f", p=128), q="pool")
                        S.dma(wd, w_down[l, e, fsl, :].rearrange("(c p) d -> p c d", p=128), q="pool")
                        for fc in range(4):
                            pg, pu = PS[fc % 2], PS[2 + fc % 2]
                            for kc in range(KC):
                                S.mm(pg[:, :], wg[:, kc, fc * 128:(fc + 1) * 128], h2b[:, kc, :],
                                     start=(kc == 0), stop=(kc == KC - 1))
                            for kc in range(KC):
                                S.mm(pu[:, :], wu[:, kc, fc * 128:(fc + 1) * 128], h2b[:, kc, :],
                                     start=(kc == 0), stop=(kc == KC - 1))
                            sg = sgs[fc % 2]
                            S.act(sg, pg[:, :], AF.Silu)
                            tmb = tms[fc % 2]
                            S.tt(tmb, pu[:, :], sg, ALU.mult)
                            S.tt(ab[fc], tmb, gb, ALU.mult)
                        for oc in range(KC):
                            py = PS[4 + oc % 4]
                            for fc in range(4):
                                S.mm(py[:, :], wd[:, fc, oc * 128:(oc + 1) * 128], ab[fc],
                                     start=(fc == 0), stop=(fc == 3))
                            if e == 0 and hf == 0:
                                S.cp(yacc[:, oc, :], py[:, :])
                            else:
                                S.tt(yacc[:, oc, :], yacc[:, oc, :], py[:, :], ALU.add)
                for oc in range(KC):
                    xc = xcs[oc % 2]
                    S.dma(xc, XT[oc, :, tsl])
                    S.stt(yacc[:, oc, :], yacc[:, oc, :], ga2[:, oc:oc + 1], xc, ALU.mult, ALU.add)
                layer_norm_fm(yacc, lng, lnb, lnb_)
                S.dma(XT[:, :, tsl].rearrange("c p t -> p c t"), yacc)
            S.barrier()
            A.release(0)

        def phase_attn(l):
            import math
            o = l * PL
            lam_init = 0.8 - 0.6 * math.exp(-0.3 * l)
            lamt = misc[:, 32:40]
            bl = pp[:, o + P_BLAM:o + P_BLAM + 256]
            prod = A.f32(128)
            S.tt(prod[:, 0:64], bl[:, 0:64], bl[:, 64:128], ALU.mult)
            S.tt(prod[:, 64:128], bl[:, 128:192], bl[:, 192:256], ALU.mult)
            S.red(lamt[:, 0:2], prod.rearrange("p (a b) -> p a b", a=2), ALU.add)
            S.act(lamt[:, 2:4], lamt[:, 0:2], AF.Exp)
            S.tt(lamt[:, 4:5], lamt[:, 2:3], lamt[:, 3:4], ALU.subtract)
            S.ts(lamt[:, 5:6], lamt[:, 4:5], -1.0, ALU.mult, -lam_init, ALU.add)
            neglam = lamt[:, 5:6]
            kTs = [A.bf(2560) for _ in range(2)]
            kT2s = [A.bf(2560) for _ in range(2)]
            for i_ in range(2):
                S.memset(kTs[i_][64:128, :], 0.0)
                S.memset(kT2s[i_][0:64, :], 0.0)
            qTs = [A.bf(2 * T) for _ in range(2)]
            vvs = [A.bf(2560).rearrange("p (b d) -> p b d", b=20) for _ in range(2)]
            pts = [A.bf(512) for _ in range(4)]
            rds = [A.f32(512) for _ in range(2)]
            ons = [A.f32(512) for _ in range(2)]
            dfs = [A.f32(256) for _ in range(2)]
            sq2 = [A.f32(256) for _ in range(2)]
            rs2 = [A.f32(256) for _ in range(2)]
            obs = [A.bf(512) for _ in range(2)]
            cnt = 0
            unit = 0
            for si in range(8):
                isB = si < 4
                if os.environ.get("DBG_ATT") and ("B" if isB else "C") not in os.environ["DBG_ATT"]:
                    continue
                if os.environ.get("DBG_ATTS") and str(si) not in os.environ["DBG_ATTS"]:
                    continue
                kT = kTs[si % 2]
                qT = qTs[si % 2]
                vv = vvs[si % 2]
                if isB:
                    h = si
                    kT2 = kT2s[si % 2]
                    S.dma(kT[0:64, :], KBd[h, 0:64, :])
                    S.dma(kT2[64:128, :], KBd[h, 64:128, :])
                    S.dma(qT[:, 0:T], QBd[h])
                    S.dma(vv, VBd[:, :, h, :].rearrange("b p d -> p b d"))
                    scale = 0.125
                else:
                    g2, pr_ = divmod(si - 4, 2)
                    h0 = g2 * 4 + pr_ * 2
                    S.dma(kT, KCd[g2])
                    S.dma(qT.rearrange("p (c t) -> p c t", c=2), QCd[h0:h0 + 2].rearrange("c p t -> p c t"))
                    S.dma(vv, VCd[:, :, g2, :].rearrange("b p d -> p b d"))
                    scale = 128.0 ** -0.5
                q3 = qT.rearrange("p (c t) -> p c t", c=2)
                for qs in range(int(os.environ.get("DBG_ATTQ", "8"))):
                    O = PS[0]
                    DN = PS[1]
                    unit += 1
                    qsl = slice(qs * 256, (qs + 1) * 256)

                    def smm(kb, sb):
                        ksl = slice(kb * 128, (kb + 1) * 128)
                        if isB:
                            S.mm(sb[:, 0:256], kT[:, ksl], qT[:, qsl])
                            S.mm(sb[:, 256:512], kT2[:, ksl], qT[:, qsl])
                        else:
                            S.mm(sb[:, :].rearrange("p (c t) -> p c t", c=2), kT[:, ksl], q3[:, :, qsl])

                    sbs = {}
                    sbs[0] = PS[4 + cnt % 4]
                    smm(0, sbs[0])
                    for kb in range(20):
                        if kb + 1 < 20:
                            sbs[kb + 1] = PS[4 + (cnt + 1) % 4]
                            smm(kb + 1, sbs[kb + 1])
                        pt = pts[cnt % 4]
                        S.act(pt, sbs[kb][:, :], AF.Exp, scale=scale, bias=mask[:, qs * 20 + kb:qs * 20 + kb + 1])
                        S.mm(O[:, :], vv[:, kb, :], pt, start=(kb == 0), stop=(kb == 19))
                        S.mm(DN[:, :], onesb, pt, start=(kb == 0), stop=(kb == 19))
                        cnt += 1
                    u2 = unit % 2
                    rd = rds[u2]
                    S.recip(rd, DN[:, :])
                    if isB and os.environ.get("DBG_BFIN") == "0":
                        pass
                    elif isB:
                        cut = int(os.environ.get("DBG_BFIN", "9"))
                        on = ons[u2]
                        S.tt(on, O[:, :], rd, ALU.mult)
                        df = dfs[u2]
                        S.stt(df, on[:, 256:512], neglam, on[:, 0:256], ALU.mult, ALU.add)
                        if cut >= 2:
                            sq = sq2[u2]
                            S.act(sq, df, AF.Square)
                            S.mm(PS[2][:, 0:256], ones, sq)
                        if cut >= 3:
                            rs = rs2[u2]
                            S.ts(rs, PS[2][:, 0:256], 1.0 / 128, ALU.mult, EPS, ALU.add)
                            S.act(rs, rs, AF.Sqrt)
                            S.recip(rs, rs)
                        if cut >= 4:
                            S.stt(df, df, pp[:, o + P_BNORM:o + P_BNORM + 1], rs, ALU.mult, ALU.mult)
                            ob = obs[u2]
                            S.ts(ob[:, 0:256], df, 1.0 - lam_init, ALU.mult)
                        if cut >= 5:
                            S.dma(OTd[4 + h, :, qsl], ob[:, 0:256])
                    else:
                        ob = obs[u2]
                        S.tt(ob, O[:, :], rd, ALU.mult)
                        S.dma(OTd[8 + h0:8 + h0 + 2, :, qsl].rearrange("c p t -> p c t"),
                              ob.rearrange("p (c t) -> p c t", c=2))
                    S.barrier()
            S.barrier()
            A.release(0)

        if stages == "all":
            for l in range(L):
                phase_proj(l)
                phase_delta(l)
                phase_attn(l)
                phase_mix_out(l)
                phase_moe(l)
        elif stages == "dbgM":
            phase_proj(0)
            phase_delta(0)
            phase_attn(0)
            phase_mix_out(0)
            dbg_x = nc.dram_tensor("dbg_x", [16, 128, T], F32, kind="ExternalOutput").ap()
            dbg_gb = nc.dram_tensor("dbg_gb", [16, 128, T], F32, kind="ExternalOutput").ap()
            dbg_h2 = nc.dram_tensor("dbg_h2", [16, 128, T], BF, kind="ExternalOutput").ap()
            S.dram_names.update(("dbg_x", "dbg_gb", "dbg_h2"))
            S.dma(dbg_x, XT)
            S.dma(dbg_gb, GBd)
            S.dma(dbg_h2, H2d)
        elif stages.startswith("dbgP"):
            phase_proj(0)
            if "A" in stages:
                phase_delta(0)
            if "T" in stages:
                phase_attn(0)
            dbg_ot = nc.dram_tensor("dbg_ot", [16, 128, T], BF, kind="ExternalOutput").ap()
            S.dram_names.add("dbg_ot")
            S.dma(dbg_ot, OTd)
        elif stages.startswith("dbg"):
            phase_proj(0, upto=int(stages[3:]))

        xfs = [A.f32(D).rearrange("p (k t) -> p k t", k=16) for _ in range(2)]
        ysg = [A.f32(D) for _ in range(2)]
        for tb in range(16):
            xf = xfs[tb % 2]
            S.dma(xf, XT[:, :, tb * 128:(tb + 1) * 128].rearrange("k p t -> p k t"))
            yo = ysg[tb % 2]
            for k4 in range(4):
                pb = PS[(tb * 4 + k4) % 4]
                for j in range(4):
                    kc = k4 * 4 + j
                    S.tr(pb[:, j * 128:(j + 1) * 128], xf[:, kc, :], ident)
                S.cp(yo[:, k4 * 512:(k4 + 1) * 512], pb[:, :], eng=("act" if k4 % 2 else "dve"))
            S.dma(y_out[tb * 128:(tb + 1) * 128, :], yo)

        S.emit(sems, dsems)
    return nc


def kernel(**inp):
    inp = {k: np.asarray(v) for k, v in inp.items()}
    nc = build_program()
    cst = _consts()
    in_maps = []
    wrg = np.ascontiguousarray(
        np.concatenate([inp["w_group"], inp["w_router"]], axis=2).reshape(L, 16, 128, 20).transpose(0, 2, 1, 3))
    shared = dict(cst=cst, w_ada=inp["w_ada"], w_in=inp["w_in"], w_out=inp["w_out"], wrg=wrg,
                  w_gate=inp["w_gate"], w_up=inp["w_up"], w_down=inp["w_down"])
    for c in range(NCORES):
        sample = c < 4
        m = dict(shared)
        if sample:
            m["x"] = np.ascontiguousarray(inp["x_sample"][c])
            cond = inp["c"][c]
            s0 = np.zeros((L, 2, 8, 4, 128, 128), np.float32)
            s0[:, 0, 0] = inp["state_a"][c, :, 0]
            s0[:, 1, 7] = inp["state_a"][c, :, 1]
            m["cache_b"] = np.ascontiguousarray(inp["cache_b_kv"][c])
            m["cache_c"] = np.ascontiguousarray(inp["cache_c_kv"][c])
        else:
            p0 = (c - 4) * 8
            m["x"] = np.ascontiguousarray(inp["x_prompt"][p0:p0 + 8].reshape(T, D))
            cond = inp["c_ctx"]
            s0 = np.zeros((L, 2, 8, 4, 128, 128), np.float32)
            m["cache_b"] = np.zeros((L, 2, 4, 512, 128), np.float32)
            m["cache_c"] = np.zeros((L, 2, 2, 512, 128), np.float32)
        m["s0"] = s0
        m["pp"] = _params(inp, cond, sample)
        m["maskb"] = _mask(sample)
        m["rope"] = _rope_tables(sample)
        in_maps.append(m)
    res = run_bass_kernel_spmd(nc, in_maps, core_ids=list(range(NCORES)))
    r = res.results
    y_s = np.stack([r[c]["y"] for c in range(4)], 0).astype(np.float32)
    y_p = np.concatenate([r[c]["y"].reshape(8, 256, D) for c in range(4, 8)], 0).astype(np.float32)
    new_a = np.concatenate([r[c]["new_a"] for c in range(4, 8)], 0).astype(np.float32)
    new_b = np.concatenate([r[c]["new_b"] for c in range(4, 8)], 0).astype(np.float32)
    new_c = np.concatenate([r[c]["new_c"] for c in range(4, 8)], 0).astype(np.float32)
    return (y_p, y_s, new_a, new_b, new_c)
```

```python
import os
import numpy as np
import concourse.bass as bass
import concourse.mybir as mybir
from concourse.bass_utils import run_bass_kernel_spmd

F32 = mybir.dt.float32
BF = mybir.dt.bfloat16
AF = mybir.ActivationFunctionType
ALU = mybir.AluOpType
AX = mybir.AxisListType

D = 2048
KC = 16
T = 2048
L = 2
IN_COLS = 5136
NCORES = 8
ALPHA = (2 * L) ** 0.25
EPS = 1e-6
ESZ = {F32: 4, BF: 2}


class Op:
    __slots__ = ("eng", "fn", "deps", "sig", "sigval", "dma", "slot", "slotval", "n")


class Sched:
    K = 8
    ENGS = ("pe", "act", "dve", "pool", "sp")

    def __init__(self, nc):
        self.nc = nc
        self.ops = {e: [] for e in self.ENGS}
        self.recs = {}
        self.ndma = {e: 0 for e in self.ENGS}
        self.notrack = set()
        self.alldma = []

    @staticmethod
    def _esz(ap):
        return ESZ.get(ap.dtype, 4)

    def box(self, ap):
        es = self._esz(ap)
        dims = list(ap.ap)
        off = ap.offset
        if ap.name in self.dram_names:
            ext = sum(abs(s) * (c - 1) for s, c in dims) + 1
            return (0, 1, off * es, (off + ext) * es)
        pstep, pcount = dims[0]
        if pstep == 0:
            pstep = 1 << 30
        p0 = off // pstep
        f0 = off % pstep
        ext = sum(abs(s) * (c - 1) for s, c in dims[1:]) + 1
        return (p0, p0 + pcount, f0 * es, (f0 + ext) * es)

    @staticmethod
    def _ov(a, b):
        return a[0] < b[1] and b[0] < a[1] and a[2] < b[3] and b[2] < a[3]

    @staticmethod
    def _cover(a, b):
        return a[0] <= b[0] and a[1] >= b[1] and a[2] <= b[2] and a[3] >= b[3]

    def add(self, eng, fn, ins, outs, dma=False):
        op = Op()
        op.eng = eng
        op.fn = fn
        op.sig = False
        op.sigval = 0
        op.dma = dma
        deps = set()
        ins = [a for a in ins if not (a is None or isinstance(a, (int, float)))]
        outs = list(outs) + [a for a in ins if a.name.startswith("ps")]
        ins = [a for a in ins if not a.name.startswith("ps")]
        for ap in ins:
            if ap is None or isinstance(ap, (int, float)) or ap.name in self.notrack:
                continue
            b = self.box(ap)
            for rec in self.recs.get(ap.name, ()):
                if self._ov(rec[0], b):
                    if rec[1] is not None:
                        deps.add(rec[1])
                    rec[2].append(op)
        for ap in outs:
            if ap.name in self.notrack:
                continue
            b = self.box(ap)
            lst = self.recs.setdefault(ap.name, [])
            keep = []
            for rec in lst:
                if self._ov(rec[0], b):
                    if rec[1] is not None:
                        deps.add(rec[1])
                    deps.update(rec[2])
                    if self._cover(b, rec[0]):
                        continue
                keep.append(rec)
            keep.append([b, op, []])
            self.recs[ap.name] = keep
        deps.discard(op)
        if eng == "pe":
            deps = {d for d in deps if not (d.eng == "pe" and not d.dma)}
        for d in deps:
            d.sig = True
        op.deps = deps
        if dma:
            op.n = self.ndma[eng]
            self.ndma[eng] += 1
            op.slot = op.n % self.K
            op.slotval = 16 * (op.n // self.K + 1)
            self.alldma.append(op)
        self.ops[eng].append(op)
        return op

    def barrier(self):
        lasts = []
        for e in self.ENGS:
            for o in reversed(self.ops[e]):
                if not o.dma and o.fn is not None:
                    lasts.append(o)
                    break
        dmas = list(self.alldma)
        self.alldma = []
        for e in self.ENGS:
            op = Op()
            op.eng = e
            op.fn = None
            op.sig = False
            op.sigval = 0
            op.dma = False
            op.deps = set(lasts) | set(dmas)
            if e == "pe":
                op.deps = {d for d in op.deps if not (d.eng == "pe" and not d.dma)}
            for d in op.deps:
                d.sig = True
            self.ops[e].append(op)
        self.recs = {}

    def mm(self, out, lhsT, rhs, start=True, stop=True):
        return self.add("pe", lambda e: e.matmul(out, lhsT, rhs, start=start, stop=stop),
                        [lhsT, rhs], [out])

    def tr(self, out, in_, ident):
        return self.add("pe", lambda e: e.transpose(out, in_, ident), [in_, ident], [out])

    def act(self, out, in_, func, scale=None, bias=None, accum_out=None):
        kw = {}
        ins = [in_]
        if scale is not None:
            kw["scale"] = scale
            if not isinstance(scale, (int, float)):
                ins.append(scale)
        if bias is not None:
            kw["bias"] = bias
            if not isinstance(bias, (int, float)):
                ins.append(bias)
        outs = [out]
        if accum_out is not None:
            kw["accum_out"] = accum_out
            outs.append(accum_out)
        return self.add("act", lambda e: e.activation(out, in_, func, **kw), ins, outs)

    def tt(self, out, a, b, op, eng="dve"):
        return self.add(eng, lambda e: e.tensor_tensor(out, a, b, op), [a, b], [out])

    def ts(self, out, a, s1, op0, s2=None, op1=None, eng="dve"):
        ins = [a] + [x for x in (s1, s2) if x is not None and not isinstance(x, (int, float))]
        if op1 is None:
            return self.add(eng, lambda e: e.tensor_scalar(out, a, s1, None, op0), ins, [out])
        return self.add(eng, lambda e: e.tensor_scalar(out, a, s1, s2, op0, op1), ins, [out])

    def stt(self, out, a, scalar, b, op0, op1):
        ins = [a, b] + ([] if isinstance(scalar, (int, float)) else [scalar])
        return self.add("dve", lambda e: e.scalar_tensor_tensor(out, a, scalar, b, op0, op1),
                        ins, [out])

    def cp(self, out, in_, eng="dve"):
        if eng == "act":
            return self.add("act", lambda e: e.copy(out, in_), [in_], [out])
        return self.add(eng, lambda e: e.tensor_copy(out, in_), [in_], [out])

    def recip(self, out, in_):
        return self.add("dve", lambda e: e.reciprocal(out, in_), [in_], [out])

    def red(self, out, in_, op):
        return self.add("dve", lambda e: e.tensor_reduce(out, in_, AX.X, op), [in_], [out])

    def memset(self, out, val, eng="dve"):
        return self.add(eng, lambda e: e.memset(out, val), [], [out])

    def dma(self, out, in_, q="sp"):
        return self.add(q, lambda e: e.dma_start(out=out, in_=in_), [in_], [out], dma=True)

    def emit(self, sems, dsems):
        nc = self.nc
        for e in self.ENGS:
            c = 0
            for op in self.ops[e]:
                if op.sig and not op.dma:
                    c += 1
                    op.sigval = c
        final = {}
        for e in self.ENGS:
            for op in self.ops[e]:
                if op.dma:
                    final[(e, op.slot)] = max(final.get((e, op.slot), 0), op.slotval)

        def replay(ename, eng):
            seen = {}
            for op in self.ops[ename]:
                need = {}
                for d in op.deps:
                    if d.dma:
                        key = ("d", d.eng, d.slot)
                        val = d.slotval
                    else:
                        key = ("e", d.eng)
                        val = d.sigval
                    if seen.get(key, 0) < val:
                        need[key] = max(need.get(key, 0), val)
                if op.dma and op.n >= self.K:
                    key = ("d", ename, op.slot)
                    val = op.slotval - 16
                    if seen.get(key, 0) < val:
                        need[key] = max(need.get(key, 0), val)
                for key, val in need.items():
                    sem = dsems[key[1]][key[2]] if key[0] == "d" else sems[key[1]]
                    eng.wait_ge(sem, val)
                    seen[key] = val
                if op.fn is None:
                    continue
                ins = op.fn(eng)
                if op.dma:
                    ins.then_inc(dsems[ename][op.slot], 16)
                elif op.sig:
                    ins.then_inc(sems[ename], 1)
            if ename == "sp":
                for (q, slot), val in final.items():
                    if seen.get(("d", q, slot), 0) < val:
                        eng.wait_ge(dsems[q][slot], val)

        with nc.Block() as blk:
            @blk.tensor
            def _(eng):
                replay("pe", eng)

            @blk.scalar
            def _(eng):
                replay("act", eng)

            @blk.vector
            def _(eng):
                replay("dve", eng)

            @blk.gpsimd
            def _(eng):
                replay("pool", eng)

            @blk.sync
            def _(eng):
                replay("sp", eng)


class Arena:
    def __init__(self, t):
        self.t = t
        self.off = 0
        self.n = t.shape[1]

    def mark(self):
        return self.off

    def release(self, m=0):
        self.off = m

    def f32(self, n):
        assert self.off + n <= self.n, ("arena overflow", self.off, n)
        a = self.t[:, self.off:self.off + n]
        self.off += n
        return a

    def bf(self, n):
        m = (n + 1) // 2
        assert self.off + m <= self.n, ("arena overflow", self.off, m)
        a = self.t[:, self.off:self.off + m].bitcast(BF)
        self.off += m
        return a


NCST = 128 + 128 + 4 * 512 + 128 + 128 + 16 * 128
C_ID, C_ONE, C_LE, C_LT, C_GE, C_GT, C_RB, C_RC, C_SEL = (
    0, 128, 256, 768, 1280, 1792, 2304, 2432, 2560)
P_CONV, P_LNG1, P_LNB1, P_LNG2, P_LNB2, P_BADA = 0, 60, 76, 92, 108, 124
P_ANORM, P_BNORM, P_CQN, P_CKN, P_CKNBC = 220, 348, 349, 350, 351
P_ALOG, P_DTB, P_BLAM, P_BGR = 479, 487, 495, 751
PL = 771
P_KEEP = 2 * PL
P_COND = 2 * PL + 1
NPP = 2 * PL + 1 + 16


def _consts():
    c = np.zeros((128, NCST), np.float32)
    a = np.arange(128)
    c[:, C_ID:C_ID + 128] = np.eye(128)
    c[:, C_ONE:C_ONE + 128] = 1.0
    le = (a[:, None] <= a[None, :]).astype(np.float32)
    lt = (a[:, None] < a[None, :]).astype(np.float32)
    ge = (a[:, None] >= a[None, :]).astype(np.float32)
    gt = (a[:, None] > a[None, :]).astype(np.float32)
    for off, m in ((C_LE, le), (C_LT, lt), (C_GE, ge), (C_GT, gt)):
        c[:, off:off + 512] = np.tile(m, (1, 4))
    rb = np.zeros((128, 128), np.float32)
    rc = np.zeros((128, 128), np.float32)
    for d in range(128):
        blk, r = divmod(d, 32)
        rb[d, blk * 32 + (r + 16) % 32] = 1.0
        blk, r = divmod(d, 64)
        rc[d, blk * 64 + (r + 32) % 64] = 1.0
    c[:, C_RB:C_RB + 128] = rb
    c[:, C_RC:C_RC + 128] = rc
    for e in range(16):
        c[e, C_SEL + e * 128:C_SEL + (e + 1) * 128] = 1.0
    return c


def _rope_tables(sample):
    tab = np.zeros((4, 128, T), np.float32)
    tab[0] = 1.0
    tab[2] = 1.0
    if not sample:
        return tab
    t = np.arange(T)
    row = (t // 64).astype(np.float32)
    col = (t % 64).astype(np.float32)

    def fill(ci, si, m):
        fr = (10000.0 ** (-np.arange(0, m, 2, dtype=np.float32) / m)).astype(np.float32)
        half = m // 2
        for d in range(128):
            blk, r = divmod(d, m)
            pos = row if blk % 2 == 0 else col
            f = fr[r % half]
            ang = (pos * f).astype(np.float32)
            tab[ci, d] = np.cos(ang)
            tab[si, d] = np.sin(ang) * (-1.0 if r < half else 1.0)

    fill(0, 1, 32)
    fill(2, 3, 64)
    return tab


def _mask(sample):
    m = np.zeros((128, 8, 20), np.float32)
    if not sample:
        m[:] = -30000.0
        for qs in range(8):
            m[:, qs, 4 + 2 * qs:4 + 2 * qs + 2] = 0.0
    return m.reshape(128, 160)


def _params(inp, cond, sample):
    p = np.zeros((128, NPP), np.float32)
    for l in range(L):
        o = l * PL
        p[:, o + P_CONV:o + P_CONV + 60] = (
            inp["a_conv"][l].reshape(5, 12, 128).transpose(2, 1, 0).reshape(128, 60))
        for k, off in ((0, P_LNG1), (1, P_LNG2)):
            p[:, o + off:o + off + 16] = inp["ln_g"][l, k].reshape(16, 128).T
        for k, off in ((0, P_LNB1), (1, P_LNB2)):
            p[:, o + off:o + off + 16] = inp["ln_b"][l, k].reshape(16, 128).T
        p[:, o + P_BADA:o + P_BADA + 96] = inp["b_ada"][l].reshape(96, 128).T
        p[:, o + P_ANORM:o + P_ANORM + 128] = inp["a_norm"][l][None, :]
        p[:, o + P_BNORM] = inp["b_norm"][l]
        p[:, o + P_CQN] = inp["c_q_norm"][l]
        p[:, o + P_CKN] = inp["c_k_norm"][l]
        p[:, o + P_CKNBC:o + P_CKNBC + 128] = inp["c_k_norm"][l][None, :]
        p[:, o + P_ALOG:o + P_ALOG + 8] = inp["a_log"][l].reshape(1, 8)
        p[:, o + P_DTB:o + P_DTB + 8] = inp["a_dt_bias"][l].reshape(1, 8)
        p[:, o + P_BLAM:o + P_BLAM + 256] = inp["b_lambda"][l].reshape(1, 256)
        p[:, o + P_BGR:o + P_BGR + 4] = inp["b_group"][l][None, :]
        p[:, o + P_BGR + 4:o + P_BGR + 20] = inp["b_router"][l][None, :]
    p[:, P_KEEP] = 1.0 if sample else 0.0
    p[:, P_COND:P_COND + 16] = cond.reshape(16, 128).T
    return p


def build_program(stages="all"):
    nc = bass.Bass("TRN2", target_bir_lowering=False)
    S = Sched(nc)

    def din(name, shape, dt=F32):
        S.notrack.add(name)
        return nc.dram_tensor(name, list(shape), dt, kind="ExternalInput").ap()

    def dout(name, shape):
        return nc.dram_tensor(name, list(shape), F32, kind="ExternalOutput").ap()

    def dscr(name, shape, dt=F32):
        return nc.dram_tensor(name, list(shape), dt, kind="Internal").ap()

    x_in = din("x", [T, D])
    cst_d = din("cst", [128, NCST])
    pp_d = din("pp", [128, NPP])
    mask_d = din("maskb", [128, 160])
    rope_d = din("rope", [4, 128, T])
    s0_d = din("s0", [L, 2, 8, 4, 128, 128])
    cb_d = din("cache_b", [L, 2, 4, 512, 128])
    cc_d = din("cache_c", [L, 2, 2, 512, 128])
    w_ada = din("w_ada", [L, D, 6 * D])
    w_in = din("w_in", [L, D, IN_COLS])
    w_out = din("w_out", [L, D, D])
    wrg_d = din("wrg", [L, 128, 16, 20])
    if stages == "all":
        w_gate = din("w_gate", [L, 16, D, 1024])
        w_up = din("w_up", [L, 16, D, 1024])
        w_down = din("w_down", [L, 16, 1024, D])

    y_out = dout("y", [T, D])
    na_out = dout("new_a", [8, L, 2, 4, 128, 128])
    nb_out = dout("new_b", [8, L, 2, 4, 256, 128])
    ncc_out = dout("new_c", [8, L, 2, 2, 256, 128])

    XT = dscr("XT", [16, 128, T])
    AQ = dscr("AQ", [12, 128, T])
    AQ2 = dscr("AQ2", [12, 128, T])
    AG = dscr("AG", [4, 128, T])
    OTd = dscr("OTd", [16, 128, T], BF)
    QBd = dscr("QBd", [4, 128, T], BF)
    KBd = dscr("KBd", [4, 128, 2560], BF)
    VBd = dscr("VBd", [20, 128, 4, 128], BF)
    QCd = dscr("QCd", [8, 128, T], BF)
    KCd = dscr("KCd", [2, 128, 2560], BF)
    VCd = dscr("VCd", [20, 128, 2, 128], BF)
    H2d = dscr("H2d", [16, 128, T], BF)
    GBd = dscr("GBd", [16, 128, T])
    S.dram_names = {"XT", "AQ", "AQ2", "AG", "OTd", "QBd", "KBd", "VBd", "QCd", "KCd", "VCd",
                    "H2d", "GBd", "y", "new_a", "new_b", "new_c"}

    ARN = 45056
    import contextlib
    with contextlib.ExitStack() as st:
        ar_t = st.enter_context(nc.sbuf_tensor("arena", [128, ARN], F32))
        cst = st.enter_context(nc.sbuf_tensor("cst_s", [128, NCST], F32))
        pp = st.enter_context(nc.sbuf_tensor("pp_s", [128, NPP], F32))
        mask = st.enter_context(nc.sbuf_tensor("mask_s", [128, 160], F32))
        md = st.enter_context(nc.sbuf_tensor("md_s", [128, L * 96], F32))
        misc = st.enter_context(nc.sbuf_tensor("misc_s", [128, 512], F32))
        onesb_t = st.enter_context(nc.sbuf_tensor("onesb_s", [128, 128], BF))
        bat_t = st.enter_context(nc.sbuf_tensor("bat_s", [128, 256], F32))
        PS = [st.enter_context(nc.psum_tensor(f"ps{i}", [128, 512], F32)) for i in range(8)]
        sems = {e: st.enter_context(nc.semaphore(f"sem_{e}")) for e in ("pe", "act", "dve", "pool")}
        dsems = {q: [st.enter_context(nc.semaphore(f"dsem_{q}{i}")) for i in range(Sched.K)]
                 for q in ("sp", "act", "pool")}

        A = Arena(ar_t)
        ident = cst[:, C_ID:C_ID + 128]
        ones = cst[:, C_ONE:C_ONE + 128]
        onesb = onesb_t[:, :]

        S.dma(cst[:, :], cst_d)
        S.dma(pp[:, :], pp_d)
        S.dma(mask[:, :], mask_d)
        S.cp(onesb, ones)
        scond = misc[:, 0:16]
        S.act(scond, pp[:, P_COND:P_COND + 16], AF.Silu)

        def mdv(l, j):
            return md[:, l * 96 + j * 16:l * 96 + (j + 1) * 16]

        if True:
            m0 = A.mark()
            modrow = A.f32(6 * D)
            wts = [A.f32(16 * 512).rearrange("p (k n) -> p k n", k=16) for _ in range(2)]
            for l in range(L):
                for ct in range(24):
                    wt = wts[ct % 2]
                    S.dma(wt, w_ada[l, :, ct * 512:(ct + 1) * 512].rearrange("(k p) n -> p k n", p=128))
                    pb = PS[ct % 2]
                    for kc in range(KC):
                        S.mm(pb[0:1, :], scond[:, kc:kc + 1], wt[:, kc, :], start=(kc == 0), stop=(kc == KC - 1))
                    S.cp(modrow[0:1, ct * 512:(ct + 1) * 512], pb[0:1, :], eng="act")
                pm = PS[2]
                for q in range(96):
                    S.mm(pm[:, q:q + 1], modrow[0:1, q * 128:(q + 1) * 128], ones[0:1, 0:1])
                o = l * PL
                S.tt(md[:, l * 96:(l + 1) * 96], pm[:, 0:96], pp[:, o + P_BADA:o + P_BADA + 96], ALU.add)
                S.ts(mdv(l, 1), mdv(l, 1), 1.0, ALU.add)
                S.ts(mdv(l, 4), mdv(l, 4), 1.0, ALU.add)
                S.ts(mdv(l, 2), mdv(l, 2), 1.0 / ALPHA, ALU.mult)
                S.ts(mdv(l, 5), mdv(l, 5), 1.0 / ALPHA, ALU.mult)
            S.barrier()
            A.release(m0)

        xts = [A.f32(D) for _ in range(2)]
        stg = [A.f32(D).rearrange("p (k t) -> p k t", k=16) for _ in range(2)]
        for tb in range(16):
            xt = xts[tb % 2]
            S.dma(xt, x_in[tb * 128:(tb + 1) * 128, :])
            sg = stg[tb % 2]
            for k4 in range(4):
                pb = PS[(tb * 4 + k4) % 4]
                for j in range(4):
                    kc = k4 * 4 + j
                    S.tr(pb[:, j * 128:(j + 1) * 128], xt[:, kc * 128:(kc + 1) * 128], ident)
                S.cp(sg[:, k4 * 4:(k4 + 1) * 4, :], pb[:, :].rearrange("p (k t) -> p k t", k=4),
                     eng=("act" if k4 % 2 else "dve"))
            S.dma(XT[:, :, tb * 128:(tb + 1) * 128].rearrange("k p t -> p k t"), sg)
        S.barrier()
        A.release(0)

        GCOLS = [0, 512, 1024, 1536, 2064, 2576, 3088, 3600, 4112, 4624]

        def phase_proj(l, upto=9):
            o = l * PL
            sh1, sc1p = mdv(l, 0), mdv(l, 1)
            hb = A.bf(16 * T).rearrange("p (k t) -> p k t", k=16)
            BAt = bat_t[:, :].rearrange("p (b c) -> p b c", b=16)
            wba = A.f32(256).rearrange("p (k c) -> p k c", k=16)
            baT = A.f32(512)
            S.dma(wba, w_in[l, :, 2048:2064].rearrange("(k p) n -> p k n", p=128))
            m2 = A.mark()
            xcs = [A.f32(512) for _ in range(3)]
            hcs = [A.f32(512) for _ in range(2)]
            for tt in range(4):
                tsl = slice(tt * 512, (tt + 1) * 512)
                pba = PS[4 + tt % 2]
                for kc in range(KC):
                    xc = xcs[(tt * 16 + kc) % 3]
                    S.dma(xc, XT[kc, :, tsl])
                    hc = hcs[kc % 2]
                    S.act(hc, xc, AF.Identity, scale=sc1p[:, kc:kc + 1], bias=sh1[:, kc:kc + 1])
                    S.cp(hb[:, kc, tsl], hc)
                    S.mm(pba[0:16, :], wba[:, kc, :], hc, start=(kc == 0), stop=(kc == KC - 1))
                S.cp(baT[0:16, :], pba[0:16, :], eng="act")
                pt = PS[6]
                for blk in range(4):
                    S.tr(pt[:, blk * 16:(blk + 1) * 16], baT[0:16, blk * 128:(blk + 1) * 128], ident[0:16, 0:16])
                S.cp(BAt[:, tt * 4:(tt + 1) * 4, :], pt[:, 0:64].rearrange("p (b c) -> p b c", b=4))
            S.barrier()
            A.release(m2)
            if upto == 1:
                A.release(0)
                return
            ck = [A.f32(512).rearrange("p (b d) -> p b d", b=4) for _ in range(2)]
            kb16 = [A.bf(512) for _ in range(2)]
            for i, (cd, nh, Kd) in enumerate(((cb_d, 4, KBd), (cc_d, 2, KCd))):
                for h in range(nh):
                    c_ = ck[h % 2]
                    S.dma(c_, cd[l, 0, h].rearrange("(b p) d -> p b d", p=128))
                    pb = PS[h % 2]
                    for b in range(4):
                        S.tr(pb[:, b * 128:(b + 1) * 128], c_[:, b, :], ident)
                    k16 = kb16[h % 2]
                    S.cp(k16, pb[:, :])
                    S.dma(Kd[h, :, 0:512], k16)
            cv = A.f32(2048).rearrange("p (b h d) -> p b h d", b=4, h=4)
            cv16 = A.bf(2048).rearrange("p (b h d) -> p b h d", b=4, h=4)
            for b in range(4):
                S.dma(cv[:, b], cb_d[l, 1, :, b * 128:(b + 1) * 128, :].rearrange("h p d -> p h d"))
            S.cp(cv16, cv)
            for b in range(4):
                S.dma(VBd[b], cv16[:, b])
            cv2 = A.f32(1024).rearrange("p (b h d) -> p b h d", b=4, h=2)
            cv216 = A.bf(1024).rearrange("p (b h d) -> p b h d", b=4, h=2)
            for b in range(4):
                S.dma(cv2[:, b], cc_d[l, 1, :, b * 128:(b + 1) * 128, :].rearrange("h p d -> p h d"))
            S.cp(cv216, cv2)
            for b in range(4):
                S.dma(VCd[b], cv216[:, b])
            S.barrier()
            A.release(m2)
            if upto == 2:
                A.release(0)
                return
            tabs = A.f32(4 * T).rearrange("p (c t) -> p c t", c=4)
            S.dma(tabs, rope_d.rearrange("c p t -> p c t"))
            wts = [A.bf(16 * 512).rearrange("p (k n) -> p k n", k=16) for _ in range(3)]
            st32 = [A.f32(512) for _ in range(3)]
            u32s = [A.f32(512) for _ in range(2)]
            sqs = [A.f32(512) for _ in range(2)]
            t1s = [A.f32(512) for _ in range(2)]
            obf = [A.bf(512) for _ in range(3)]
            tks = [A.f32(512) for _ in range(2)]
            tk16 = [A.bf(512) for _ in range(2)]
            sm = A.f32(8)
            cnt = 0
            for g in range(10):
                wt = wts[g % 3]
                S.dma(wt, w_in[l, :, GCOLS[g]:GCOLS[g] + 512].rearrange("(k p) n -> p k n", p=128), q="pool")
                for j in range(4):
                    ci = g * 4 + j
                    if g == 6 or (g == 9 and j >= 2):
                        continue
                    if os.environ.get("DBG_NOFM") and ci >= int(os.environ["DBG_NOFM"]):
                        continue
                    for tt in range(4):
                        tsl = slice(tt * 512, (tt + 1) * 512)
                        cnt += 1
                        pb = PS[cnt % 3]
                        for kc in range(KC):
                            S.mm(pb[:, :], wt[:, kc, j * 128:(j + 1) * 128], hb[:, kc, tsl],
                                 start=(kc == 0), stop=(kc == KC - 1))
                        if ci < 12:
                            s_ = st32[cnt % 3]
                            S.cp(s_, pb[:, :], eng="act")
                            S.dma(AQ[ci, :, tsl], s_)
                            continue
                        if ci < 16:
                            s_ = st32[cnt % 3]
                            S.act(s_, pb[:, :], AF.Silu)
                            S.dma(AG[ci - 12, :, tsl], s_)
                            continue
                        isB = ci < 28
                        u = u32s[cnt % 2]
                        S.cp(u, pb[:, :], eng="act")
                        if not isB:
                            sq = sqs[cnt % 2]
                            S.act(sq, pb[:, :], AF.Square)
                            pr = PS[3]
                            S.mm(pr[:, :], ones, sq)
                            rs = t1s[cnt % 2]
                            S.ts(rs, pr[:, :], 1.0 / 128, ALU.mult, EPS, ALU.add)
                            S.act(rs, rs, AF.Sqrt)
                            S.recip(rs, rs)
                            wcol = pp[:, o + P_CQN:o + P_CQN + 1] if ci < 36 else pp[:, o + P_CKN:o + P_CKN + 1]
                            S.stt(u, u, wcol, rs, ALU.mult, ALU.mult)
                        R = cst[:, C_RB:C_RB + 128] if isB else cst[:, C_RC:C_RC + 128]
                        ti = 0 if isB else 2
                        pr2 = PS[4]
                        S.mm(pr2[:, :], R, u)
                        t1 = t1s[(cnt + 1) % 2] if not isB else t1s[cnt % 2]
                        S.tt(t1, pr2[:, :], tabs[:, ti + 1, tsl], ALU.mult)
                        S.tt(u, u, tabs[:, ti, tsl], ALU.mult)
                        ob = obf[cnt % 3]
                        S.tt(ob, u, t1, ALU.add)
                        if ci < 20:
                            dst = QBd[ci - 16, :, tsl]
                        elif ci < 24:
                            dst = KBd[ci - 20, :, 512 + tt * 512:512 + (tt + 1) * 512]
                        elif ci < 36:
                            dst = QCd[ci - 28, :, tsl]
                        else:
                            dst = KCd[ci - 36, :, 512 + tt * 512:512 + (tt + 1) * 512]
                        S.dma(dst, ob)
                if g in (5, 6, 9) and not os.environ.get("DBG_NOTM") and str(g) in os.environ.get("DBG_TMG", "569"):
                    for tb in range(16):
                        pb = PS[5 + tb % 2]
                        for kc in range(KC):
                            S.mm(pb[:, :], hb[:, kc, tb * 128:(tb + 1) * 128], wt[:, kc, :],
                                 start=(kc == 0), stop=(kc == KC - 1))
                        seg, tl = tb // 2, (tb % 2) * 128
                        s_ = tks[tb % 2]
                        if g in (5, 6):
                            S.cp(s_, pb[:, :], eng="act")
                            S.dma(nb_out[seg, l, g - 5, :, tl:tl + 128, :].rearrange("h t d -> t h d"),
                                  s_.rearrange("p (h d) -> p h d", h=4))
                            if g == 6:
                                v16 = tk16[tb % 2]
                                S.cp(v16, pb[:, :])
                                S.dma(VBd[4 + tb], v16.rearrange("p (h d) -> p h d", h=4))
                        else:
                            for g2 in range(2):
                                S.act(sqs[g2][:, 0:128], pb[:, g2 * 128:(g2 + 1) * 128], AF.Square,
                                      accum_out=sm[:, g2:g2 + 1])
                            S.ts(sm[:, 2:4], sm[:, 0:2], 1.0 / 128, ALU.mult, EPS, ALU.add)
                            S.act(sm[:, 2:4], sm[:, 2:4], AF.Sqrt)
                            S.recip(sm[:, 4:6], sm[:, 2:4])
                            for g2 in range(2):
                                S.stt(s_[:, g2 * 128:(g2 + 1) * 128], pb[:, g2 * 128:(g2 + 1) * 128],
                                      sm[:, 4 + g2:5 + g2], pp[:, o + P_CKNBC:o + P_CKNBC + 128],
                                      ALU.mult, ALU.mult)
                            S.cp(s_[:, 256:512], pb[:, 256:512], eng="act")
                            S.dma(ncc_out[seg, l, 0, :, tl:tl + 128, :].rearrange("h t d -> t h d"),
                                  s_[:, 0:256].rearrange("p (h d) -> p h d", h=2))
                            S.dma(ncc_out[seg, l, 1, :, tl:tl + 128, :].rearrange("h t d -> t h d"),
                                  s_[:, 256:512].rearrange("p (h d) -> p h d", h=2))
                            v16 = tk16[tb % 2]
                            S.cp(v16[:, 0:256], pb[:, 256:512])
                            S.dma(VCd[4 + tb], v16[:, 0:256].rearrange("p (h d) -> p h d", h=2))
            S.barrier()
            A.release(0)
            return BAt

        def phase_delta(l, DB):
            o = l * PL
            BAt = bat_t[:, :].rearrange("p (b c) -> p b c", b=16)
            keep = pp[:, P_KEEP:P_KEEP + 1]
            pad = A.f32(8 * 260).rearrange("p (s c) -> p s c", s=8)
            cv = A.f32(T)
            cv3 = cv.rearrange("p (s c) -> p s c", s=8)
            sqb = A.f32(T)
            rsb = A.f32(512)
            for ci in range(12):
                S.dma(pad[:, :, 2:258], AQ[ci].rearrange("p (s c) -> p s c", s=8))
                S.memset(pad[:, 0:1, 0:2], 0.0)
                S.memset(pad[:, 7:8, 258:260], 0.0)
                S.ts(pad[:, 1:8, 0:2], pad[:, 0:7, 256:258], keep, ALU.mult)
                S.ts(pad[:, 0:7, 258:260], pad[:, 1:8, 2:4], keep, ALU.mult)

                def w(j):
                    c0 = o + P_CONV + ci * 5 + j
                    return pp[:, c0:c0 + 1]
                S.ts(cv3, pad[:, :, 0:256], w(0), ALU.mult)
                for j in range(1, 5):
                    S.stt(cv3, pad[:, :, j:j + 256], w(j), cv3, ALU.mult, ALU.add)
                S.act(cv, cv, AF.Silu)
                if ci < 8:
                    S.act(sqb, cv, AF.Square)
                    for tt in range(4):
                        tsl = slice(tt * 512, (tt + 1) * 512)
                        pr = PS[tt % 2]
                        S.mm(pr[:, :], ones, sqb[:, tsl])
                        S.ts(rsb, pr[:, :], EPS, ALU.add)
                        S.act(rsb, rsb, AF.Sqrt)
                        S.recip(rsb, rsb)
                        if ci < 4:
                            S.stt(cv[:, tsl], cv[:, tsl], 128.0 ** -0.5, rsb, ALU.mult, ALU.mult)
                        else:
                            S.tt(cv[:, tsl], cv[:, tsl], rsb, ALU.mult)
                S.dma(AQ2[ci], cv)
            S.barrier()
            A.release(0)
            Bt = A.f32(128).rearrange("p (b c) -> p b c", b=16)
            Gt = A.f32(128).rearrange("p (b c) -> p b c", b=16)
            nA = A.f32(8)
            S.act(nA, pp[:, o + P_ALOG:o + P_ALOG + 8], AF.Exp)
            S.ts(nA, nA, -1.0, ALU.mult)
            S.act(Bt, BAt[:, :, 0:8], AF.Sigmoid)
            S.tt(Gt, BAt[:, :, 8:16], pp[:, o + P_DTB:o + P_DTB + 8].unsqueeze(1).to_broadcast([128, 16, 8]), ALU.add)
            S.act(Gt, Gt, AF.Exp)
            S.act(Gt, Gt, AF.Ln, bias=1.0)
            S.tt(Gt, Gt, nA.unsqueeze(1).to_broadcast([128, 16, 8]), ALU.mult)
            def B512():
                return A.f32(512)

            def v3(a):
                return a.rearrange("p (h j) -> p h j", h=4)

            def hs(a, h):
                return a[:, h * 128:(h + 1) * 128]
            qkv = A.f32(12 * 128).rearrange("p (c t) -> p c t", c=12)
            gm, Dsb, E, t1, qk, N, NT, qkT, RT = (B512() for _ in range(9))
            Pa, PTa, Pb, PTb = (B512() for _ in range(4))
            vb, kdec, rhs2, dl, qss, s0t, Irep = (B512() for _ in range(7))
            Sst = [B512() for _ in range(2)]
            otok = A.f32(16 * 512).rearrange("p (b x) -> p b x", b=16)
            sc12 = A.f32(12)
            nb = A.f32(4)
            bx = A.f32(4)
            S.tt(Irep, cst[:, C_LE:C_LE + 512], cst[:, C_GE:C_GE + 512], ALU.mult)
            for dr in range(2):
                Uin, Lst, Linc, Lstr = (C_LE, C_GT, C_GE, C_GT) if dr == 0 else (C_GE, C_LT, C_LE, C_LT)
                St = Sst[dr]
                S.memset(St, 0.0)
                order = range(16) if dr == 0 else range(15, -1, -1)
                for tb in order:
                    seg = tb // 2
                    is_start = (tb % 2 == 0) if dr == 0 else (tb % 2 == 1)
                    g4 = Gt[:, tb, dr * 4:dr * 4 + 4]
                    b4 = Bt[:, tb, dr * 4:dr * 4 + 4]

                    def bc(a):
                        return a.unsqueeze(2).to_broadcast([128, 4, 128])
                    S.dma(qkv, AQ2[:, :, tb * 128:(tb + 1) * 128].rearrange("c p t -> p c t"))
                    if is_start:
                        S.dma(v3(s0t), s0_d[l, dr, seg].rearrange("h k v -> k h v"))
                        S.stt(St, St, keep, s0t, ALU.mult, ALU.add)
                    S.tt(v3(gm), v3(cst[:, Lst:Lst + 512]), bc(g4), ALU.mult)
                    S.mm(DB[0][:, :], cst[:, Uin:Uin + 128], gm)
                    S.mm(DB[1][:, 0:4], cst[:, Uin:Uin + 128], g4)
                    S.mm(DB[1][:, 4:8], cst[:, Lst:Lst + 128], g4)
                    S.mm(DB[1][:, 8:12], ones, g4)
                    S.cp(Dsb, DB[0][:, :])
                    S.act(E, Dsb, AF.Exp)
                    S.cp(sc12, DB[1][:, 0:12])
                    S.act(sc12, sc12, AF.Exp)
                    yield 1
                    eg, er, ge = sc12[:, 0:4], sc12[:, 4:8], sc12[:, 8:12]
                    for h in range(4):
                        S.mm(hs(DB[2], h), qkv[:, h, :], qkv[:, 4 + h, :])
                        S.mm(hs(DB[3], h), qkv[:, 4 + h, :], qkv[:, 4 + h, :])
                    for h in range(4):
                        S.tr(hs(DB[4], h), qkv[:, 4 + h, :], ident)
                        S.tr(hs(DB[5], h), qkv[:, 8 + h, :], ident)
                    S.tt(t1, E, cst[:, Linc:Linc + 512], ALU.mult)
                    S.tt(qk, DB[2][:, :], t1, ALU.mult)
                    S.tt(t1, E, cst[:, Lstr:Lstr + 512], ALU.mult)
                    S.tt(N, DB[3][:, :], t1, ALU.mult)
                    S.ts(nb, b4, -1.0, ALU.mult)
                    S.tt(v3(N), v3(N), bc(nb), ALU.mult)
                    S.tt(bx, b4, eg, ALU.mult)
                    S.tt(v3(kdec), v3(DB[4][:, :]), bc(er), ALU.mult)
                    S.tt(v3(vb), v3(DB[5][:, :]), bc(b4), ALU.mult)
                    yield 1
                    for h in range(4):
                        S.tr(hs(DB[6], h), hs(N, h), ident)
                        S.tr(hs(DB[7], h), hs(qk, h), ident)
                    S.cp(NT, DB[6][:, :])
                    S.cp(qkT, DB[7][:, :])
                    S.tt(RT, NT, Irep, ALU.add)
                    yield 1
                    P_, PT_ = N, NT
                    for stg in range(6):
                        Pn, PTn = (Pa, PTa) if stg % 2 == 0 else (Pb, PTb)
                        for h in range(4):
                            S.mm(hs(DB[0], h), hs(PT_, h), hs(P_, h))
                        if stg < 5:
                            for h in range(4):
                                S.mm(hs(DB[1], h), hs(P_, h), hs(PT_, h))
                        yield 1
                        S.cp(Pn, DB[0][:, :])
                        if stg < 5:
                            S.cp(PTn, DB[1][:, :])
                        for h in range(4):
                            S.mm(hs(DB[2], h), hs(Pn, h), hs(RT, h))
                        S.tt(RT, RT, DB[2][:, :], ALU.add)
                        yield 1
                        P_, PT_ = Pn, PTn
                    for h in range(4):
                        S.mm(hs(DB[3], h), qkv[:, 4 + h, :], hs(St, h))
                        S.mm(hs(DB[4], h), qkv[:, h, :], hs(St, h))
                    S.tt(v3(t1), v3(DB[3][:, :]), bc(bx), ALU.mult)
                    S.tt(rhs2, vb, t1, ALU.subtract)
                    S.tt(v3(qss), v3(DB[4][:, :]), bc(eg), ALU.mult)
                    yield 1
                    for h in range(4):
                        S.mm(hs(DB[5], h), hs(RT, h), hs(rhs2, h))
                    S.cp(dl, DB[5][:, :])
                    yield 1
                    for h in range(4):
                        S.mm(hs(DB[6], h), hs(qkT, h), hs(dl, h))
                        S.mm(hs(DB[7], h), hs(kdec, h), hs(dl, h))
                    if dr == 0:
                        S.tt(otok[:, tb, :], DB[6][:, :], qss, ALU.add)
                    else:
                        S.tt(t1, DB[6][:, :], qss, ALU.add)
                        S.tt(otok[:, tb, :], otok[:, tb, :], t1, ALU.add)
                    for h in range(4):
                        S.stt(hs(St, h), hs(St, h), ge[:, h:h + 1], hs(DB[7], h), ALU.mult, ALU.add)
                    if not is_start:
                        S.dma(na_out[seg, l, dr].rearrange("h k v -> k h v"), v3(St))
                    yield 1
            yield 0
            ss = A.f32(8)
            junk = A.f32(128)
            on = A.f32(512)
            ag = [A.f32(512) for _ in range(2)]
            ob4 = [A.bf(512) for _ in range(2)]
            for tb in range(16):
                ot = otok[:, tb, :]
                for h in range(4):
                    S.act(junk, hs(ot, h), AF.Square, accum_out=ss[:, h:h + 1])
                S.ts(ss[:, 4:8], ss[:, 0:4], 1.0 / 128, ALU.mult, EPS, ALU.add)
                S.act(ss[:, 4:8], ss[:, 4:8], AF.Sqrt)
                S.recip(ss[:, 4:8], ss[:, 4:8])
                for h in range(4):
                    S.stt(hs(on, h), hs(ot, h), ss[:, 4 + h:5 + h], pp[:, o + P_ANORM:o + P_ANORM + 128],
                          ALU.mult, ALU.mult)
                pb = PS[tb % 2]
                for h in range(4):
                    S.tr(hs(pb, h), hs(on, h), ident)
                a_ = ag[tb % 2]
                S.dma(v3(a_), AG[:, :, tb * 128:(tb + 1) * 128].rearrange("c p t -> p c t"))
                ob = ob4[tb % 2]
                S.tt(ob, pb[:, :], a_, ALU.mult)
                S.dma(OTd[0:4, :, tb * 128:(tb + 1) * 128].rearrange("c p t -> p c t"), v3(ob))
            S.barrier()
            A.release(0)

        def layer_norm_fm(xt, gcol, bcol, bufs):
            sqs_, mean, msq, var = bufs
            pm, pq = PS[2], PS[3]
            for kc in range(KC):
                sqc = sqs_[kc % 2]
                S.act(sqc, xt[:, kc, :], AF.Square)
                S.mm(pm[:, :], ones, xt[:, kc, :], start=(kc == 0), stop=(kc == KC - 1))
                S.mm(pq[:, :], ones, sqc, start=(kc == 0), stop=(kc == KC - 1))
            S.ts(mean, pm[:, :], 1.0 / D, ALU.mult)
            S.tt(msq, mean, mean, ALU.mult)
            S.stt(var, pq[:, :], 1.0 / D, msq, ALU.mult, ALU.subtract)
            S.ts(var, var, EPS / (ALPHA * ALPHA), ALU.add)
            S.act(var, var, AF.Sqrt)
            S.recip(var, var)
            S.tt(xt, xt, mean.unsqueeze(1).to_broadcast([128, 16, 512]), ALU.subtract)
            S.tt(xt, xt, var.unsqueeze(1).to_broadcast([128, 16, 512]), ALU.mult)
            for kc in range(KC):
                S.act(xt[:, kc, :], xt[:, kc, :], AF.Identity, scale=gcol[:, kc:kc + 1], bias=bcol[:, kc:kc + 1])

        def phase_mix_out(l):
            o = l * PL
            ga1, sh2, sc2p = mdv(l, 2), mdv(l, 3), mdv(l, 4)
            lng = pp[:, o + P_LNG1:o + P_LNG1 + 16]
            lnb = pp[:, o + P_LNB1:o + P_LNB1 + 16]
            wo = A.bf(16 * D).rearrange("p (k n) -> p k n", k=16)
            for c4 in range(4):
                S.dma(wo[:, :, c4 * 512:(c4 + 1) * 512],
                      w_out[l, :, c4 * 512:(c4 + 1) * 512].rearrange("(k p) n -> p k n", p=128), q="pool")
            wrg = A.f32(320).rearrange("p (k c) -> p k c", k=16)
            S.dma(wrg, wrg_d[l])
            lg = A.f32(320).rearrange("p (b c) -> p b c", b=16)
            lgT = A.f32(512)
            ots = [A.bf(16 * 512).rearrange("p (k t) -> p k t", k=16) for _ in range(1)]
            xt = A.f32(16 * 512).rearrange("p (k t) -> p k t", k=16)
            h2b = A.bf(16 * 512).rearrange("p (k t) -> p k t", k=16)
            h2c = [A.f32(512) for _ in range(2)]
            lnb_ = ([A.f32(512) for _ in range(2)], A.f32(512), A.f32(512), A.f32(512))
            for tt in range(4):
                tsl = slice(tt * 512, (tt + 1) * 512)
                ot = ots[0]
                S.dma(ot, OTd[:, :, tsl].rearrange("c p t -> p c t"))
                S.dma(xt, XT[:, :, tsl].rearrange("c p t -> p c t"))
                for oc in range(KC):
                    pb = PS[oc % 2]
                    for kc in range(KC):
                        S.mm(pb[:, :], wo[:, kc, oc * 128:(oc + 1) * 128], ot[:, kc, :],
                             start=(kc == 0), stop=(kc == KC - 1))
                    S.stt(xt[:, oc, :], pb[:, :], ga1[:, oc:oc + 1], xt[:, oc, :], ALU.mult, ALU.add)
                layer_norm_fm(xt, lng, lnb, lnb_)
                S.dma(XT[:, :, tsl].rearrange("c p t -> p c t"), xt)
                pr = PS[4]
                for kc in range(KC):
                    hc = h2c[kc % 2]
                    S.act(hc, xt[:, kc, :], AF.Identity, scale=sc2p[:, kc:kc + 1], bias=sh2[:, kc:kc + 1])
                    S.cp(h2b[:, kc, :], hc)
                    S.mm(pr[0:20, :], wrg[:, kc, :], hc, start=(kc == 0), stop=(kc == KC - 1))
                S.dma(H2d[:, :, tsl].rearrange("c p t -> p c t"), h2b)
                S.cp(lgT[0:20, :], pr[0:20, :])
                pt = PS[5]
                for blk in range(4):
                    S.tr(pt[:, blk * 20:(blk + 1) * 20], lgT[0:20, blk * 128:(blk + 1) * 128], ident[0:20, 0:20])
                S.tt(lg[:, tt * 4:(tt + 1) * 4, :], pt[:, 0:80].rearrange("p (b c) -> p b c", b=4),
                     pp[:, o + P_BGR:o + P_BGR + 20].unsqueeze(1).to_broadcast([128, 4, 20]), ALU.add)
            def sm(n):
                return A.f32(n)
            gl = lg[:, :, 0:4]
            el4 = lg[:, :, 4:20].rearrange("p b (g e) -> p b g e", g=4)
            gmax, gsum, gv, m1, m2, d21, w1, w2 = (sm(16) for _ in range(8))
            ohg, gex, sel, oh1, msk, oh2, ge4, tmp4 = (sm(64).rearrange("p (b c) -> p b c", b=16) for _ in range(8))
            prod = sm(256).rearrange("p (b g e) -> p b g e", b=16, g=4)
            gates = sm(256)
            gates4 = gates.rearrange("p (b g e) -> p b g e", b=16, g=4)

            def b3(a):
                return a.unsqueeze(2).to_broadcast([128, 16, 4])
            S.red(gmax, gl, ALU.max)
            S.tt(ohg, gl, b3(gmax), ALU.is_equal)
            S.tt(gex, gl, b3(gmax), ALU.subtract)
            S.act(gex, gex, AF.Exp)
            S.red(gsum, gex, ALU.add)
            S.recip(gv, gsum)
            S.tt(prod, el4, ohg.unsqueeze(3).to_broadcast([128, 16, 4, 4]), ALU.mult)
            S.red(sel, prod.rearrange("p b g e -> p b e g"), ALU.add)
            S.red(m1, sel, ALU.max)
            S.tt(oh1, sel, b3(m1), ALU.is_equal)
            S.stt(msk, oh1, -1.0e30, sel, ALU.mult, ALU.add)
            S.red(m2, msk, ALU.max)
            S.tt(oh2, msk, b3(m2), ALU.is_equal)
            S.tt(d21, m2, m1, ALU.subtract)
            S.act(d21, d21, AF.Exp)
            S.ts(w1, d21, 1.0, ALU.add)
            S.recip(w1, w1)
            S.tt(w2, d21, w1, ALU.mult)
            S.tt(w1, w1, gv, ALU.mult)
            S.tt(w2, w2, gv, ALU.mult)
            S.tt(ge4, oh1, b3(w1), ALU.mult)
            S.tt(tmp4, oh2, b3(w2), ALU.mult)
            S.tt(ge4, ge4, tmp4, ALU.add)
            S.tt(gates4, ohg.unsqueeze(3).to_broadcast([128, 16, 4, 4]),
                 ge4.unsqueeze(2).to_broadcast([128, 16, 4, 4]), ALU.mult)
            gT = A.f32(T)
            for tb in range(16):
                pb = PS[6 + (tb // 4) % 2]
                S.tr(pb[0:16, (tb % 4) * 128:(tb % 4 + 1) * 128], gates[:, tb * 16:(tb + 1) * 16], ident)
                if tb % 4 == 3:
                    S.cp(gT[0:16, (tb // 4) * 512:(tb // 4 + 1) * 512], pb[0:16, :])
            gbs = [A.f32(512) for _ in range(3)]
            for e in range(16):
                for tt in range(4):
                    tsl = slice(tt * 512, (tt + 1) * 512)
                    pb = PS[(e * 4 + tt) % 2]
                    S.mm(pb[:, :], cst[0:16, C_SEL + e * 128:C_SEL + (e + 1) * 128], gT[0:16, tsl])
                    gb = gbs[(e * 4 + tt) % 3]
                    S.cp(gb, pb[:, :])
                    S.dma(GBd[e, :, tsl], gb)
            S.barrier()
            A.release(0)

        def phase_moe(l):
            o = l * PL
            ga2 = mdv(l, 5)
            lng = pp[:, o + P_LNG2:o + P_LNG2 + 16]
            lnb = pp[:, o + P_LNB2:o + P_LNB2 + 16]
            h2b = A.bf(16 * 512).rearrange("p (k t) -> p k t", k=16)
            yacc = A.f32(16 * 512).rearrange("p (k t) -> p k t", k=16)
            wgs = [A.bf(16 * 512).rearrange("p (k f) -> p k f", k=16) for _ in range(2)]
            wus = [A.bf(16 * 512).rearrange("p (k f) -> p k f", k=16) for _ in range(2)]
            wds = [A.bf(4 * D).rearrange("p (c d) -> p c d", c=4) for _ in range(2)]
            gbs = [A.f32(512) for _ in range(2)]
            sgs = [A.f32(512) for _ in range(2)]
            tms = [A.f32(512) for _ in range(2)]
            actb = [[A.bf(512) for _ in range(4)] for _ in range(2)]
            xcs = [A.f32(512) for _ in range(2)]
            lnb_ = (sgs, tms[0], tms[1], gbs[0])
            ui = 0
            for tm in range(4):
                tsl = slice(tm * 512, (tm + 1) * 512)
                S.dma(h2b, H2d[:, :, tsl].rearrange("c p t -> p c t"))
                for e in range(int(os.environ.get("DBG_NEXP", "16"))):
                    gb = gbs[e % 2]
                    S.dma(gb, GBd[e, :, tsl])
                    for hf in range(2):
                        wg, wu, wd = wgs[ui % 2], wus[ui % 2], wds[ui % 2]
                        ab = actb[ui % 2]
                        ui += 1
                        fsl = slice(hf * 512, (hf + 1) * 512)
                        S.dma(wg, w_gate[l, e, :, fsl].rearrange("(k p) f -> p k f", p=128), q="pool")
                        S.dma(wu, w_up[l, e, :, fsl].rearrange("(k p) f -> p k f", p=128), q="pool")
                        S.dma(wd, w_down[l, e, fsl, :].rearrange("(c p) d -> p c d", p=128), q="pool")
                        for fc in range(4):
                            pg, pu = PS[fc % 2], PS[2 + fc % 2]
                            for kc in range(KC):
                                S.mm(pg[:, :], wg[:, kc, fc * 128:(fc + 1) * 128], h2b[:, kc, :],
                                     start=(kc == 0), stop=(kc == KC - 1))
                            for kc in range(KC):
                                S.mm(pu[:, :], wu[:, kc, fc * 128:(fc + 1) * 128], h2b[:, kc, :],
                                     start=(kc == 0), stop=(kc == KC - 1))
                            sg = sgs[fc % 2]
                            S.act(sg, pg[:, :], AF.Silu)
                            tmb = tms[fc % 2]
                            S.tt(tmb, pu[:, :], sg, ALU.mult)
                            S.tt(ab[fc], tmb, gb, ALU.mult)
                        for oc in range(KC):
                            py = PS[4 + oc % 4]
                            for fc in range(4):
                                S.mm(py[:, :], wd[:, fc, oc * 128:(oc + 1) * 128], ab[fc],
                                     start=(fc == 0), stop=(fc == 3))
                            if e == 0 and hf == 0:
                                S.cp(yacc[:, oc, :], py[:, :])
                            else:
                                S.tt(yacc[:, oc, :], yacc[:, oc, :], py[:, :], ALU.add)
                for oc in range(KC):
                    xc = xcs[oc % 2]
                    S.dma(xc, XT[oc, :, tsl])
                    S.stt(yacc[:, oc, :], yacc[:, oc, :], ga2[:, oc:oc + 1], xc, ALU.mult, ALU.add)
                layer_norm_fm(yacc, lng, lnb, lnb_)
                S.dma(XT[:, :, tsl].rearrange("c p t -> p c t"), yacc)
            S.barrier()
            A.release(0)

        def phase_attn(l, coop=False):
            import math
            o = l * PL
            lam_init = 0.8 - 0.6 * math.exp(-0.3 * l)
            lamt = misc[:, 32:40]
            bl = pp[:, o + P_BLAM:o + P_BLAM + 256]
            prod = A.f32(128)
            S.tt(prod[:, 0:64], bl[:, 0:64], bl[:, 64:128], ALU.mult)
            S.tt(prod[:, 64:128], bl[:, 128:192], bl[:, 192:256], ALU.mult)
            S.red(lamt[:, 0:2], prod.rearrange("p (a b) -> p a b", a=2), ALU.add)
            S.act(lamt[:, 2:4], lamt[:, 0:2], AF.Exp)
            S.tt(lamt[:, 4:5], lamt[:, 2:3], lamt[:, 3:4], ALU.subtract)
            S.ts(lamt[:, 5:6], lamt[:, 4:5], -1.0, ALU.mult, -lam_init, ALU.add)
            neglam = lamt[:, 5:6]
            kTs = [A.bf(2560) for _ in range(2)]
            kT2s = [A.bf(2560) for _ in range(2)]
            for i_ in range(2):
                S.memset(kTs[i_][64:128, :], 0.0)
                S.memset(kT2s[i_][0:64, :], 0.0)
            qTs = [A.bf(2 * T) for _ in range(2)]
            vvs = [A.bf(2560).rearrange("p (b d) -> p b d", b=20) for _ in range(2)]
            pts = [A.bf(512) for _ in range(4)]
            rds = [A.f32(512) for _ in range(2)]
            ons = [A.f32(512) for _ in range(2)]
            dfs = [A.f32(256) for _ in range(2)]
            sq2 = [A.f32(256) for _ in range(2)]
            rs2 = [A.f32(256) for _ in range(2)]
            obs = [A.bf(512) for _ in range(2)]
            cnt = 0
            unit = 0
            for si in range(8):
                isB = si < 4
                if os.environ.get("DBG_ATT") and ("B" if isB else "C") not in os.environ["DBG_ATT"]:
                    continue
                if os.environ.get("DBG_ATTS") and str(si) not in os.environ["DBG_ATTS"]:
                    continue
                kT = kTs[si % 2]
                qT = qTs[si % 2]
                vv = vvs[si % 2]
                if isB:
                    h = si
                    kT2 = kT2s[si % 2]
                    S.dma(kT[0:64, :], KBd[h, 0:64, :])
                    S.dma(kT2[64:128, :], KBd[h, 64:128, :])
                    S.dma(qT[:, 0:T], QBd[h])
                    S.dma(vv, VBd[:, :, h, :].rearrange("b p d -> p b d"))
                    scale = 0.125
                else:
                    g2, pr_ = divmod(si - 4, 2)
                    h0 = g2 * 4 + pr_ * 2
                    S.dma(kT, KCd[g2])
                    S.dma(qT.rearrange("p (c t) -> p c t", c=2), QCd[h0:h0 + 2].rearrange("c p t -> p c t"))
                    S.dma(vv, VCd[:, :, g2, :].rearrange("b p d -> p b d"))
                    scale = 128.0 ** -0.5
                q3 = qT.rearrange("p (c t) -> p c t", c=2)
                for qs in range(int(os.environ.get("DBG_ATTQ", "8"))):
                    O = PS[4] if coop else PS[unit % 2]
                    DN = PS[5] if coop else PS[2 + unit % 2]
                    unit += 1
                    qsl = slice(qs * 256, (qs + 1) * 256)

                    def smm(kb, sb):
                        ksl = slice(kb * 128, (kb + 1) * 128)
                        if isB:
                            S.mm(sb[:, 0:256], kT[:, ksl], qT[:, qsl])
                            S.mm(sb[:, 256:512], kT2[:, ksl], qT[:, qsl])
                        else:
                            S.mm(sb[:, :].rearrange("p (c t) -> p c t", c=2), kT[:, ksl], q3[:, :, qsl])

                    sbs = {}
                    def sbank(i_):
                        return PS[6 + i_ % 2] if coop else PS[4 + i_ % 4]
                    sbs[0] = sbank(cnt)
                    smm(0, sbs[0])
                    for kb in range(20):
                        if kb + 1 < 20:
                            sbs[kb + 1] = sbank(cnt + 1)
                            smm(kb + 1, sbs[kb + 1])
                        pt = pts[cnt % 4]
                        S.act(pt, sbs[kb][:, :], AF.Exp, scale=scale, bias=mask[:, qs * 20 + kb:qs * 20 + kb + 1])
                        S.mm(O[:, :], vv[:, kb, :], pt, start=(kb == 0), stop=(kb == 19))
                        S.mm(DN[:, :], onesb, pt, start=(kb == 0), stop=(kb == 19))
                        cnt += 1
                        yield 1
                    u2 = unit % 2
                    rd = rds[u2]
                    S.cp(rd, DN[:, :])
                    S.recip(rd, rd)
                    if isB and os.environ.get("DBG_BFIN") == "0":
                        pass
                    elif isB:
                        cut = int(os.environ.get("DBG_BFIN", "9"))
                        on = ons[u2]
                        S.tt(on, O[:, :], rd, ALU.mult)
                        df = dfs[u2]
                        S.stt(df, on[:, 256:512], neglam, on[:, 0:256], ALU.mult, ALU.add)
                        if cut >= 2:
                            sq = sq2[u2]
                            S.act(sq, df, AF.Square)
                            S.mm(DN[:, 0:256], ones, sq)
                        if cut >= 3:
                            rs = rs2[u2]
                            S.ts(rs, DN[:, 0:256], 1.0 / 128, ALU.mult, EPS, ALU.add)
                            S.act(rs, rs, AF.Sqrt)
                            S.recip(rs, rs)
                        if cut >= 4:
                            S.stt(df, df, pp[:, o + P_BNORM:o + P_BNORM + 1], rs, ALU.mult, ALU.mult)
                            ob = obs[u2]
                            S.ts(ob[:, 0:256], df, 1.0 - lam_init, ALU.mult)
                        if cut >= 5:
                            S.dma(OTd[4 + h, :, qsl], ob[:, 0:256])
                    else:
                        ob = obs[u2]
                        S.tt(ob, O[:, :], rd, ALU.mult)
                        S.dma(OTd[8 + h0:8 + h0 + 2, :, qsl].rearrange("c p t -> p c t"),
                              ob.rearrange("p (c t) -> p c t", c=2))
            if not coop:
                S.barrier()
                A.release(0)

        def run_gen(g):
            for _ in g:
                pass

        def phase_mixers(l):
            dg = phase_delta(l, [PS[0], PS[1], PS[2], PS[3], PS[0], PS[1], PS[2], PS[3]])
            ag = None
            d_loop_done = False
            a_done = False
            while True:
                if not d_loop_done:
                    r = next(dg)
                    if r == 0:
                        d_loop_done = True
                if ag is None:
                    ag = phase_attn(l, coop=True)
                if not a_done:
                    for _ in range(3):
                        try:
                            next(ag)
                        except StopIteration:
                            a_done = True
                            break
                if d_loop_done and a_done:
                    break
            run_gen(dg)

        if stages == "all":
            for l in range(L):
                phase_proj(l)
                phase_mixers(l)
                phase_mix_out(l)
                phase_moe(l)
        elif stages == "dbgM":
            phase_proj(0)
            phase_mixers(0)
            phase_mix_out(0)
            dbg_x = nc.dram_tensor("dbg_x", [16, 128, T], F32, kind="ExternalOutput").ap()
            dbg_gb = nc.dram_tensor("dbg_gb", [16, 128, T], F32, kind="ExternalOutput").ap()
            dbg_h2 = nc.dram_tensor("dbg_h2", [16, 128, T], BF, kind="ExternalOutput").ap()
            S.dram_names.update(("dbg_x", "dbg_gb", "dbg_h2"))
            S.dma(dbg_x, XT)
            S.dma(dbg_gb, GBd)
            S.dma(dbg_h2, H2d)
        elif stages.startswith("dbgP"):
            phase_proj(0)
            if "X" in stages:
                phase_mixers(0)
            else:
                if "A" in stages:
                    run_gen(phase_delta(0, PS))
                if "T" in stages:
                    run_gen(phase_attn(0))
            dbg_ot = nc.dram_tensor("dbg_ot", [16, 128, T], BF, kind="ExternalOutput").ap()
            S.dram_names.add("dbg_ot")
            S.dma(dbg_ot, OTd)
        elif stages.startswith("dbg"):
            phase_proj(0, upto=int(stages[3:]))

        xfs = [A.f32(D).rearrange("p (k t) -> p k t", k=16) for _ in range(2)]
        ysg = [A.f32(D) for _ in range(2)]
        for tb in range(16):
            xf = xfs[tb % 2]
            S.dma(xf, XT[:, :, tb * 128:(tb + 1) * 128].rearrange("k p t -> p k t"))
            yo = ysg[tb % 2]
            for k4 in range(4):
                pb = PS[(tb * 4 + k4) % 4]
                for j in range(4):
                    kc = k4 * 4 + j
                    S.tr(pb[:, j * 128:(j + 1) * 128], xf[:, kc, :], ident)
                S.cp(yo[:, k4 * 512:(k4 + 1) * 512], pb[:, :], eng=("act" if k4 % 2 else "dve"))
            S.dma(y_out[tb * 128:(tb + 1) * 128, :], yo)

        S.emit(sems, dsems)
    return nc


def kernel(**inp):
    inp = {k: np.asarray(v) for k, v in inp.items()}
    nc = build_program()
    cst = _consts()
    in_maps = []
    wrg = np.ascontiguousarray(
        np.concatenate([inp["w_group"], inp["w_router"]], axis=2).reshape(L, 16, 128, 20).transpose(0, 2, 1, 3))
    shared = dict(cst=cst, w_ada=inp["w_ada"], w_in=inp["w_in"], w_out=inp["w_out"], wrg=wrg,
                  w_gate=inp["w_gate"], w_up=inp["w_up"], w_down=inp["w_down"])
    for c in range(NCORES):
        sample = c < 4
        m = dict(shared)
        if sample:
            m["x"] = np.ascontiguousarray(inp["x_sample"][c])
            cond = inp["c"][c]
            s0 = np.zeros((L, 2, 8, 4, 128, 128), np.float32)
            s0[:, 0, 0] = inp["state_a"][c, :, 0]
            s0[:, 1, 7] = inp["state_a"][c, :, 1]
            m["cache_b"] = np.ascontiguousarray(inp["cache_b_kv"][c])
            m["cache_c"] = np.ascontiguousarray(inp["cache_c_kv"][c])
        else:
            p0 = (c - 4) * 8
            m["x"] = np.ascontiguousarray(inp["x_prompt"][p0:p0 + 8].reshape(T, D))
            cond = inp["c_ctx"]
            s0 = np.zeros((L, 2, 8, 4, 128, 128), np.float32)
            m["cache_b"] = np.zeros((L, 2, 4, 512, 128), np.float32)
            m["cache_c"] = np.zeros((L, 2, 2, 512, 128), np.float32)
        m["s0"] = s0
        m["pp"] = _params(inp, cond, sample)
        m["maskb"] = _mask(sample)
        m["rope"] = _rope_tables(sample)
        in_maps.append(m)
    res = run_bass_kernel_spmd(nc, in_maps, core_ids=list(range(NCORES)))
    r = res.results
    y_s = np.stack([r[c]["y"] for c in range(4)], 0).astype(np.float32)
    y_p = np.concatenate([r[c]["y"].reshape(8, 256, D) for c in range(4, 8)], 0).astype(np.float32)
    new_a = np.concatenate([r[c]["new_a"] for c in range(4, 8)], 0).astype(np.float32)
    new_b = np.concatenate([r[c]["new_b"] for c in range(4, 8)], 0).astype(np.float32)
    new_c = np.concatenate([r[c]["new_c"] for c in range(4, 8)], 0).astype(np.float32)
    return (y_p, y_s, new_a, new_b, new_c)
```

```python
import os
import numpy as np
import concourse.bass as bass
import concourse.mybir as mybir
from concourse.bass_utils import run_bass_kernel_spmd

F32 = mybir.dt.float32
BF = mybir.dt.bfloat16
AF = mybir.ActivationFunctionType
ALU = mybir.AluOpType
AX = mybir.AxisListType

D = 2048
KC = 16
T = 2048
L = 2
IN_COLS = 5136
NCORES = 8
ALPHA = (2 * L) ** 0.25
EPS = 1e-6
ESZ = {F32: 4, BF: 2}


class Op:
    __slots__ = ("eng", "fn", "deps", "sig", "sigval", "dma", "slot", "slotval", "n")


class Sched:
    K = 8
    ENGS = ("pe", "act", "dve", "pool", "sp")

    def __init__(self, nc):
        self.nc = nc
        self.ops = {e: [] for e in self.ENGS}
        self.recs = {}
        self.ndma = {e: 0 for e in self.ENGS}
        self.notrack = set()
        self.alldma = []

    @staticmethod
    def _esz(ap):
        return ESZ.get(ap.dtype, 4)

    def box(self, ap):
        es = self._esz(ap)
        dims = list(ap.ap)
        off = ap.offset
        if ap.name in self.dram_names:
            ext = sum(abs(s) * (c - 1) for s, c in dims) + 1
            return (0, 1, off * es, (off + ext) * es)
        pstep, pcount = dims[0]
        if pstep == 0:
            pstep = 1 << 30
        p0 = off // pstep
        f0 = off % pstep
        ext = sum(abs(s) * (c - 1) for s, c in dims[1:]) + 1
        return (p0, p0 + pcount, f0 * es, (f0 + ext) * es)

    @staticmethod
    def _ov(a, b):
        return a[0] < b[1] and b[0] < a[1] and a[2] < b[3] and b[2] < a[3]

    @staticmethod
    def _cover(a, b):
        return a[0] <= b[0] and a[1] >= b[1] and a[2] <= b[2] and a[3] >= b[3]

    def add(self, eng, fn, ins, outs, dma=False):
        op = Op()
        op.eng = eng
        op.fn = fn
        op.sig = False
        op.sigval = 0
        op.dma = dma
        deps = set()
        ins = [a for a in ins if not (a is None or isinstance(a, (int, float)))]
        outs = list(outs) + [a for a in ins if a.name.startswith("ps")]
        ins = [a for a in ins if not a.name.startswith("ps")]
        for ap in ins:
            if ap is None or isinstance(ap, (int, float)) or ap.name in self.notrack:
                continue
            b = self.box(ap)
            for rec in self.recs.get(ap.name, ()):
                if self._ov(rec[0], b):
                    if rec[1] is not None:
                        deps.add(rec[1])
                    rec[2].append(op)
        for ap in outs:
            if ap.name in self.notrack:
                continue
            b = self.box(ap)
            lst = self.recs.setdefault(ap.name, [])
            keep = []
            for rec in lst:
                if self._ov(rec[0], b):
                    if rec[1] is not None:
                        deps.add(rec[1])
                    deps.update(rec[2])
                    if self._cover(b, rec[0]):
                        continue
                keep.append(rec)
            keep.append([b, op, []])
            self.recs[ap.name] = keep
        deps.discard(op)
        if eng == "pe":
            deps = {d for d in deps if not (d.eng == "pe" and not d.dma)}
        for d in deps:
            d.sig = True
        op.deps = deps
        if dma:
            op.n = self.ndma[eng]
            self.ndma[eng] += 1
            op.slot = op.n % self.K
            op.slotval = 16 * (op.n // self.K + 1)
            self.alldma.append(op)
        self.ops[eng].append(op)
        return op

    def barrier(self):
        lasts = []
        for e in self.ENGS:
            for o in reversed(self.ops[e]):
                if not o.dma and o.fn is not None:
                    lasts.append(o)
                    break
        dmas = list(self.alldma)
        self.alldma = []
        for e in self.ENGS:
            op = Op()
            op.eng = e
            op.fn = None
            op.sig = False
            op.sigval = 0
            op.dma = False
            op.deps = set(lasts) | set(dmas)
            if e == "pe":
                op.deps = {d for d in op.deps if not (d.eng == "pe" and not d.dma)}
            for d in op.deps:
                d.sig = True
            self.ops[e].append(op)
        self.recs = {}

    def mm(self, out, lhsT, rhs, start=True, stop=True):
        return self.add("pe", lambda e: e.matmul(out, lhsT, rhs, start=start, stop=stop),
                        [lhsT, rhs], [out])

    def tr(self, out, in_, ident):
        return self.add("pe", lambda e: e.transpose(out, in_, ident), [in_, ident], [out])

    def act(self, out, in_, func, scale=None, bias=None, accum_out=None):
        kw = {}
        ins = [in_]
        if scale is not None:
            kw["scale"] = scale
            if not isinstance(scale, (int, float)):
                ins.append(scale)
        if bias is not None:
            kw["bias"] = bias
            if not isinstance(bias, (int, float)):
                ins.append(bias)
        outs = [out]
        if accum_out is not None:
            kw["accum_out"] = accum_out
            outs.append(accum_out)
        return self.add("act", lambda e: e.activation(out, in_, func, **kw), ins, outs)

    def tt(self, out, a, b, op, eng="dve"):
        return self.add(eng, lambda e: e.tensor_tensor(out, a, b, op), [a, b], [out])

    def ts(self, out, a, s1, op0, s2=None, op1=None, eng="dve"):
        ins = [a] + [x for x in (s1, s2) if x is not None and not isinstance(x, (int, float))]
        if op1 is None:
            return self.add(eng, lambda e: e.tensor_scalar(out, a, s1, None, op0), ins, [out])
        return self.add(eng, lambda e: e.tensor_scalar(out, a, s1, s2, op0, op1), ins, [out])

    def stt(self, out, a, scalar, b, op0, op1):
        ins = [a, b] + ([] if isinstance(scalar, (int, float)) else [scalar])
        return self.add("dve", lambda e: e.scalar_tensor_tensor(out, a, scalar, b, op0, op1),
                        ins, [out])

    def cp(self, out, in_, eng="dve"):
        if eng == "act":
            return self.add("act", lambda e: e.copy(out, in_), [in_], [out])
        return self.add(eng, lambda e: e.tensor_copy(out, in_), [in_], [out])

    def recip(self, out, in_):
        return self.add("dve", lambda e: e.reciprocal(out, in_), [in_], [out])

    def red(self, out, in_, op):
        return self.add("dve", lambda e: e.tensor_reduce(out, in_, AX.X, op), [in_], [out])

    def memset(self, out, val, eng="dve"):
        return self.add(eng, lambda e: e.memset(out, val), [], [out])

    def dma(self, out, in_, q="sp"):
        return self.add(q, lambda e: e.dma_start(out=out, in_=in_), [in_], [out], dma=True)

    def emit(self, sems, dsems):
        nc = self.nc
        for e in self.ENGS:
            c = 0
            for op in self.ops[e]:
                if op.sig and not op.dma:
                    c += 1
                    op.sigval = c
        final = {}
        for e in self.ENGS:
            for op in self.ops[e]:
                if op.dma:
                    final[(e, op.slot)] = max(final.get((e, op.slot), 0), op.slotval)

        def replay(ename, eng):
            seen = {}
            for op in self.ops[ename]:
                need = {}
                for d in op.deps:
                    if d.dma:
                        key = ("d", d.eng, d.slot)
                        val = d.slotval
                    else:
                        key = ("e", d.eng)
                        val = d.sigval
                    if seen.get(key, 0) < val:
                        need[key] = max(need.get(key, 0), val)
                if op.dma and op.n >= self.K:
                    key = ("d", ename, op.slot)
                    val = op.slotval - 16
                    if seen.get(key, 0) < val:
                        need[key] = max(need.get(key, 0), val)
                for key, val in need.items():
                    sem = dsems[key[1]][key[2]] if key[0] == "d" else sems[key[1]]
                    eng.wait_ge(sem, val)
                    seen[key] = val
                if op.fn is None:
                    continue
                ins = op.fn(eng)
                if op.dma:
                    ins.then_inc(dsems[ename][op.slot], 16)
                elif op.sig:
                    ins.then_inc(sems[ename], 1)
            if ename == "sp":
                for (q, slot), val in final.items():
                    if seen.get(("d", q, slot), 0) < val:
                        eng.wait_ge(dsems[q][slot], val)

        with nc.Block() as blk:
            @blk.tensor
            def _(eng):
                replay("pe", eng)

            @blk.scalar
            def _(eng):
                replay("act", eng)

            @blk.vector
            def _(eng):
                replay("dve", eng)

            @blk.gpsimd
            def _(eng):
                replay("pool", eng)

            @blk.sync
            def _(eng):
                replay("sp", eng)


class Arena:
    def __init__(self, t):
        self.t = t
        self.off = 0
        self.n = t.shape[1]

    def mark(self):
        return self.off

    def release(self, m=0):
        self.off = m

    def f32(self, n):
        assert self.off + n <= self.n, ("arena overflow", self.off, n)
        a = self.t[:, self.off:self.off + n]
        self.off += n
        return a

    def bf(self, n):
        m = (n + 1) // 2
        assert self.off + m <= self.n, ("arena overflow", self.off, m)
        a = self.t[:, self.off:self.off + m].bitcast(BF)
        self.off += m
        return a


NCST = 128 + 128 + 4 * 512 + 128 + 128 + 16 * 128
C_ID, C_ONE, C_LE, C_LT, C_GE, C_GT, C_RB, C_RC, C_SEL = (
    0, 128, 256, 768, 1280, 1792, 2304, 2432, 2560)
P_CONV, P_LNG1, P_LNB1, P_LNG2, P_LNB2, P_BADA = 0, 60, 76, 92, 108, 124
P_ANORM, P_BNORM, P_CQN, P_CKN, P_CKNBC = 220, 348, 349, 350, 351
P_ALOG, P_DTB, P_BLAM, P_BGR = 479, 487, 495, 751
PL = 771
P_KEEP = 2 * PL
P_COND = 2 * PL + 1
NPP = 2 * PL + 1 + 16


def _consts():
    c = np.zeros((128, NCST), np.float32)
    a = np.arange(128)
    c[:, C_ID:C_ID + 128] = np.eye(128)
    c[:, C_ONE:C_ONE + 128] = 1.0
    le = (a[:, None] <= a[None, :]).astype(np.float32)
    lt = (a[:, None] < a[None, :]).astype(np.float32)
    ge = (a[:, None] >= a[None, :]).astype(np.float32)
    gt = (a[:, None] > a[None, :]).astype(np.float32)
    for off, m in ((C_LE, le), (C_LT, lt), (C_GE, ge), (C_GT, gt)):
        c[:, off:off + 512] = np.tile(m, (1, 4))
    rb = np.zeros((128, 128), np.float32)
    rc = np.zeros((128, 128), np.float32)
    for d in range(128):
        blk, r = divmod(d, 32)
        rb[d, blk * 32 + (r + 16) % 32] = 1.0
        blk, r = divmod(d, 64)
        rc[d, blk * 64 + (r + 32) % 64] = 1.0
    c[:, C_RB:C_RB + 128] = rb
    c[:, C_RC:C_RC + 128] = rc
    for e in range(16):
        c[e, C_SEL + e * 128:C_SEL + (e + 1) * 128] = 1.0
    return c


def _rope_tables(sample):
    tab = np.zeros((4, 128, T), np.float32)
    tab[0] = 1.0
    tab[2] = 1.0
    if not sample:
        return tab
    t = np.arange(T)
    row = (t // 64).astype(np.float32)
    col = (t % 64).astype(np.float32)

    def fill(ci, si, m):
        fr = (10000.0 ** (-np.arange(0, m, 2, dtype=np.float32) / m)).astype(np.float32)
        half = m // 2
        for d in range(128):
            blk, r = divmod(d, m)
            pos = row if blk % 2 == 0 else col
            f = fr[r % half]
            ang = (pos * f).astype(np.float32)
            tab[ci, d] = np.cos(ang)
            tab[si, d] = np.sin(ang) * (-1.0 if r < half else 1.0)

    fill(0, 1, 32)
    fill(2, 3, 64)
    return tab


def _mask(sample):
    m = np.zeros((128, 8, 20), np.float32)
    if not sample:
        m[:] = -30000.0
        for qs in range(8):
            m[:, qs, 4 + 2 * qs:4 + 2 * qs + 2] = 0.0
    return m.reshape(128, 160)


def _params(inp, cond, sample):
    p = np.zeros((128, NPP), np.float32)
    for l in range(L):
        o = l * PL
        p[:, o + P_CONV:o + P_CONV + 60] = (
            inp["a_conv"][l].reshape(5, 12, 128).transpose(2, 1, 0).reshape(128, 60))
        for k, off in ((0, P_LNG1), (1, P_LNG2)):
            p[:, o + off:o + off + 16] = inp["ln_g"][l, k].reshape(16, 128).T
        for k, off in ((0, P_LNB1), (1, P_LNB2)):
            p[:, o + off:o + off + 16] = inp["ln_b"][l, k].reshape(16, 128).T
        p[:, o + P_BADA:o + P_BADA + 96] = inp["b_ada"][l].reshape(96, 128).T
        p[:, o + P_ANORM:o + P_ANORM + 128] = inp["a_norm"][l][None, :]
        p[:, o + P_BNORM] = inp["b_norm"][l]
        p[:, o + P_CQN] = inp["c_q_norm"][l]
        p[:, o + P_CKN] = inp["c_k_norm"][l]
        p[:, o + P_CKNBC:o + P_CKNBC + 128] = inp["c_k_norm"][l][None, :]
        p[:, o + P_ALOG:o + P_ALOG + 8] = inp["a_log"][l].reshape(1, 8)
        p[:, o + P_DTB:o + P_DTB + 8] = inp["a_dt_bias"][l].reshape(1, 8)
        p[:, o + P_BLAM:o + P_BLAM + 256] = inp["b_lambda"][l].reshape(1, 256)
        p[:, o + P_BGR:o + P_BGR + 4] = inp["b_group"][l][None, :]
        p[:, o + P_BGR + 4:o + P_BGR + 20] = inp["b_router"][l][None, :]
    p[:, P_KEEP] = 1.0 if sample else 0.0
    p[:, P_COND:P_COND + 16] = cond.reshape(16, 128).T
    return p


def build_program(stages="all"):
    nc = bass.Bass("TRN2", target_bir_lowering=False)
    S = Sched(nc)

    def din(name, shape, dt=F32):
        S.notrack.add(name)
        return nc.dram_tensor(name, list(shape), dt, kind="ExternalInput").ap()

    def dout(name, shape):
        return nc.dram_tensor(name, list(shape), F32, kind="ExternalOutput").ap()

    def dscr(name, shape, dt=F32):
        return nc.dram_tensor(name, list(shape), dt, kind="Internal").ap()

    x_in = din("x", [T, D])
    cst_d = din("cst", [128, NCST])
    pp_d = din("pp", [128, NPP])
    mask_d = din("maskb", [128, 160])
    rope_d = din("rope", [4, 128, T])
    s0_d = din("s0", [L, 2, 8, 4, 128, 128])
    cb_d = din("cache_b", [L, 2, 4, 512, 128])
    cc_d = din("cache_c", [L, 2, 2, 512, 128])
    w_ada = din("w_ada", [L, D, 6 * D])
    w_in = din("w_in", [L, D, IN_COLS])
    w_out = din("w_out", [L, D, D])
    wrg_d = din("wrg", [L, 128, 16, 20])
    if stages == "all":
        w_gate = din("w_gate", [L, 16, D, 1024])
        w_up = din("w_up", [L, 16, D, 1024])
        w_down = din("w_down", [L, 16, 1024, D])

    y_out = dout("y", [T, D])
    na_out = dout("new_a", [8, L, 2, 4, 128, 128])
    nb_out = dout("new_b", [8, L, 2, 4, 256, 128])
    ncc_out = dout("new_c", [8, L, 2, 2, 256, 128])

    XT = dscr("XT", [16, 128, T])
    AQ = dscr("AQ", [12, 128, T])
    AQ2 = dscr("AQ2", [12, 128, T])
    AG = dscr("AG", [4, 128, T])
    OTd = dscr("OTd", [16, 128, T], BF)
    QBd = dscr("QBd", [4, 128, T], BF)
    KBd = dscr("KBd", [4, 128, 2560], BF)
    VBd = dscr("VBd", [20, 128, 4, 128], BF)
    QCd = dscr("QCd", [8, 128, T], BF)
    KCd = dscr("KCd", [2, 128, 2560], BF)
    VCd = dscr("VCd", [20, 128, 2, 128], BF)
    H2d = dscr("H2d", [16, 128, T], BF)
    GBd = dscr("GBd", [16, 128, T])
    S.dram_names = {"XT", "AQ", "AQ2", "AG", "OTd", "QBd", "KBd", "VBd", "QCd", "KCd", "VCd",
                    "H2d", "GBd", "y", "new_a", "new_b", "new_c"}

    ARN = 45056
    import contextlib
    with contextlib.ExitStack() as st:
        ar_t = st.enter_context(nc.sbuf_tensor("arena", [128, ARN], F32))
        cst = st.enter_context(nc.sbuf_tensor("cst_s", [128, NCST], F32))
        pp = st.enter_context(nc.sbuf_tensor("pp_s", [128, NPP], F32))
        mask = st.enter_context(nc.sbuf_tensor("mask_s", [128, 160], F32))
        md = st.enter_context(nc.sbuf_tensor("md_s", [128, L * 96], F32))
        misc = st.enter_context(nc.sbuf_tensor("misc_s", [128, 512], F32))
        onesb_t = st.enter_context(nc.sbuf_tensor("onesb_s", [128, 128], BF))
        bat_t = st.enter_context(nc.sbuf_tensor("bat_s", [128, 256], F32))
        PS = [st.enter_context(nc.psum_tensor(f"ps{i}", [128, 512], F32)) for i in range(8)]
        sems = {e: st.enter_context(nc.semaphore(f"sem_{e}")) for e in ("pe", "act", "dve", "pool")}
        dsems = {q: [st.enter_context(nc.semaphore(f"dsem_{q}{i}")) for i in range(Sched.K)]
                 for q in ("sp", "act", "pool")}

        A = Arena(ar_t)
        ident = cst[:, C_ID:C_ID + 128]
        ones = cst[:, C_ONE:C_ONE + 128]
        onesb = onesb_t[:, :]

        S.dma(cst[:, :], cst_d)
        S.dma(pp[:, :], pp_d)
        S.dma(mask[:, :], mask_d)
        S.cp(onesb, ones)
        scond = misc[:, 0:16]
        S.act(scond, pp[:, P_COND:P_COND + 16], AF.Silu)

        def mdv(l, j):
            return md[:, l * 96 + j * 16:l * 96 + (j + 1) * 16]

        if True:
            m0 = A.mark()
            modrow = A.f32(6 * D)
            wts = [A.f32(16 * 512).rearrange("p (k n) -> p k n", k=16) for _ in range(2)]
            for l in range(L):
                for ct in range(24):
                    wt = wts[ct % 2]
                    S.dma(wt, w_ada[l, :, ct * 512:(ct + 1) * 512].rearrange("(k p) n -> p k n", p=128))
                    pb = PS[ct % 2]
                    for kc in range(KC):
                        S.mm(pb[0:1, :], scond[:, kc:kc + 1], wt[:, kc, :], start=(kc == 0), stop=(kc == KC - 1))
                    S.cp(modrow[0:1, ct * 512:(ct + 1) * 512], pb[0:1, :], eng="act")
                pm = PS[2]
                for q in range(96):
                    S.mm(pm[:, q:q + 1], modrow[0:1, q * 128:(q + 1) * 128], ones[0:1, 0:1])
                o = l * PL
                S.tt(md[:, l * 96:(l + 1) * 96], pm[:, 0:96], pp[:, o + P_BADA:o + P_BADA + 96], ALU.add)
                S.ts(mdv(l, 1), mdv(l, 1), 1.0, ALU.add)
                S.ts(mdv(l, 4), mdv(l, 4), 1.0, ALU.add)
                S.ts(mdv(l, 2), mdv(l, 2), 1.0 / ALPHA, ALU.mult)
                S.ts(mdv(l, 5), mdv(l, 5), 1.0 / ALPHA, ALU.mult)
            S.barrier()
            A.release(m0)

        xts = [A.f32(D) for _ in range(2)]
        stg = [A.f32(D).rearrange("p (k t) -> p k t", k=16) for _ in range(2)]
        for tb in range(16):
            xt = xts[tb % 2]
            S.dma(xt, x_in[tb * 128:(tb + 1) * 128, :])
            sg = stg[tb % 2]
            for k4 in range(4):
                pb = PS[(tb * 4 + k4) % 4]
                for j in range(4):
                    kc = k4 * 4 + j
                    S.tr(pb[:, j * 128:(j + 1) * 128], xt[:, kc * 128:(kc + 1) * 128], ident)
                S.cp(sg[:, k4 * 4:(k4 + 1) * 4, :], pb[:, :].rearrange("p (k t) -> p k t", k=4),
                     eng=("act" if k4 % 2 else "dve"))
            S.dma(XT[:, :, tb * 128:(tb + 1) * 128].rearrange("k p t -> p k t"), sg)
        S.barrier()
        A.release(0)

        GCOLS = [0, 512, 1024, 1536, 2064, 2576, 3088, 3600, 4112, 4624]

        def phase_proj(l, upto=9):
            o = l * PL
            sh1, sc1p = mdv(l, 0), mdv(l, 1)
            hb = A.bf(16 * T).rearrange("p (k t) -> p k t", k=16)
            BAt = bat_t[:, :].rearrange("p (b c) -> p b c", b=16)
            wba = A.f32(256).rearrange("p (k c) -> p k c", k=16)
            baT = A.f32(512)
            S.dma(wba, w_in[l, :, 2048:2064].rearrange("(k p) n -> p k n", p=128))
            m2 = A.mark()
            xcs = [A.f32(512) for _ in range(3)]
            hcs = [A.f32(512) for _ in range(2)]
            for tt in range(4):
                tsl = slice(tt * 512, (tt + 1) * 512)
                pba = PS[4 + tt % 2]
                for kc in range(KC):
                    xc = xcs[(tt * 16 + kc) % 3]
                    S.dma(xc, XT[kc, :, tsl])
                    hc = hcs[kc % 2]
                    S.act(hc, xc, AF.Identity, scale=sc1p[:, kc:kc + 1], bias=sh1[:, kc:kc + 1])
                    S.cp(hb[:, kc, tsl], hc)
                    S.mm(pba[0:16, :], wba[:, kc, :], hc, start=(kc == 0), stop=(kc == KC - 1))
                S.cp(baT[0:16, :], pba[0:16, :], eng="act")
                pt = PS[6]
                for blk in range(4):
                    S.tr(pt[:, blk * 16:(blk + 1) * 16], baT[0:16, blk * 128:(blk + 1) * 128], ident[0:16, 0:16])
                S.cp(BAt[:, tt * 4:(tt + 1) * 4, :], pt[:, 0:64].rearrange("p (b c) -> p b c", b=4))
            S.barrier()
            A.release(m2)
            if upto == 1:
                A.release(0)
                return
            ck = [A.f32(512).rearrange("p (b d) -> p b d", b=4) for _ in range(2)]
            kb16 = [A.bf(512) for _ in range(2)]
            for i, (cd, nh, Kd) in enumerate(((cb_d, 4, KBd), (cc_d, 2, KCd))):
                for h in range(nh):
                    c_ = ck[h % 2]
                    S.dma(c_, cd[l, 0, h].rearrange("(b p) d -> p b d", p=128))
                    pb = PS[h % 2]
                    for b in range(4):
                        S.tr(pb[:, b * 128:(b + 1) * 128], c_[:, b, :], ident)
                    k16 = kb16[h % 2]
                    S.cp(k16, pb[:, :])
                    S.dma(Kd[h, :, 0:512], k16)
            cv = A.f32(2048).rearrange("p (b h d) -> p b h d", b=4, h=4)
            cv16 = A.bf(2048).rearrange("p (b h d) -> p b h d", b=4, h=4)
            for b in range(4):
                S.dma(cv[:, b], cb_d[l, 1, :, b * 128:(b + 1) * 128, :].rearrange("h p d -> p h d"))
            S.cp(cv16, cv)
            for b in range(4):
                S.dma(VBd[b], cv16[:, b])
            cv2 = A.f32(1024).rearrange("p (b h d) -> p b h d", b=4, h=2)
            cv216 = A.bf(1024).rearrange("p (b h d) -> p b h d", b=4, h=2)
            for b in range(4):
                S.dma(cv2[:, b], cc_d[l, 1, :, b * 128:(b + 1) * 128, :].rearrange("h p d -> p h d"))
            S.cp(cv216, cv2)
            for b in range(4):
                S.dma(VCd[b], cv216[:, b])
            S.barrier()
            A.release(m2)
            if upto == 2:
                A.release(0)
                return
            tabs = A.f32(4 * T).rearrange("p (c t) -> p c t", c=4)
            S.dma(tabs, rope_d.rearrange("c p t -> p c t"))
            wts = [A.bf(16 * 512).rearrange("p (k n) -> p k n", k=16) for _ in range(3)]
            st32 = [A.f32(512) for _ in range(3)]
            u32s = [A.f32(512) for _ in range(2)]
            sqs = [A.f32(512) for _ in range(2)]
            t1s = [A.f32(512) for _ in range(2)]
            obf = [A.bf(512) for _ in range(3)]
            tks = [A.f32(512) for _ in range(2)]
            tk16 = [A.bf(512) for _ in range(2)]
            sm = A.f32(8)
            cnt = 0
            for g in range(10):
                wt = wts[g % 3]
                S.dma(wt, w_in[l, :, GCOLS[g]:GCOLS[g] + 512].rearrange("(k p) n -> p k n", p=128), q="pool")
                for j in range(4):
                    ci = g * 4 + j
                    if g == 6 or (g == 9 and j >= 2):
                        continue
                    if os.environ.get("DBG_NOFM") and ci >= int(os.environ["DBG_NOFM"]):
                        continue
                    for tt in range(4):
                        tsl = slice(tt * 512, (tt + 1) * 512)
                        cnt += 1
                        pb = PS[cnt % 3]
                        for kc in range(KC):
                            S.mm(pb[:, :], wt[:, kc, j * 128:(j + 1) * 128], hb[:, kc, tsl],
                                 start=(kc == 0), stop=(kc == KC - 1))
                        if ci < 12:
                            s_ = st32[cnt % 3]
                            S.cp(s_, pb[:, :], eng="act")
                            S.dma(AQ[ci, :, tsl], s_)
                            continue
                        if ci < 16:
                            s_ = st32[cnt % 3]
                            S.act(s_, pb[:, :], AF.Silu)
                            S.dma(AG[ci - 12, :, tsl], s_)
                            continue
                        isB = ci < 28
                        u = u32s[cnt % 2]
                        S.cp(u, pb[:, :], eng="act")
                        if not isB:
                            sq = sqs[cnt % 2]
                            S.act(sq, pb[:, :], AF.Square)
                            pr = PS[3]
                            S.mm(pr[:, :], ones, sq)
                            rs = t1s[cnt % 2]
                            S.ts(rs, pr[:, :], 1.0 / 128, ALU.mult, EPS, ALU.add)
                            S.act(rs, rs, AF.Sqrt)
                            S.recip(rs, rs)
                            wcol = pp[:, o + P_CQN:o + P_CQN + 1] if ci < 36 else pp[:, o + P_CKN:o + P_CKN + 1]
                            S.stt(u, u, wcol, rs, ALU.mult, ALU.mult)
                        R = cst[:, C_RB:C_RB + 128] if isB else cst[:, C_RC:C_RC + 128]
                        ti = 0 if isB else 2
                        pr2 = PS[4]
                        S.mm(pr2[:, :], R, u)
                        t1 = t1s[(cnt + 1) % 2] if not isB else t1s[cnt % 2]
                        S.tt(t1, pr2[:, :], tabs[:, ti + 1, tsl], ALU.mult)
                        S.tt(u, u, tabs[:, ti, tsl], ALU.mult)
                        ob = obf[cnt % 3]
                        S.tt(ob, u, t1, ALU.add)
                        if ci < 20:
                            dst = QBd[ci - 16, :, tsl]
                        elif ci < 24:
                            dst = KBd[ci - 20, :, 512 + tt * 512:512 + (tt + 1) * 512]
                        elif ci < 36:
                            dst = QCd[ci - 28, :, tsl]
                        else:
                            dst = KCd[ci - 36, :, 512 + tt * 512:512 + (tt + 1) * 512]
                        S.dma(dst, ob)
                if g in (5, 6, 9) and not os.environ.get("DBG_NOTM") and str(g) in os.environ.get("DBG_TMG", "569"):
                    for tb in range(16):
                        pb = PS[5 + tb % 2]
                        for kc in range(KC):
                            S.mm(pb[:, :], hb[:, kc, tb * 128:(tb + 1) * 128], wt[:, kc, :],
                                 start=(kc == 0), stop=(kc == KC - 1))
                        seg, tl = tb // 2, (tb % 2) * 128
                        s_ = tks[tb % 2]
                        if g in (5, 6):
                            S.cp(s_, pb[:, :], eng="act")
                            S.dma(nb_out[seg, l, g - 5, :, tl:tl + 128, :].rearrange("h t d -> t h d"),
                                  s_.rearrange("p (h d) -> p h d", h=4))
                            if g == 6:
                                v16 = tk16[tb % 2]
                                S.cp(v16, pb[:, :])
                                S.dma(VBd[4 + tb], v16.rearrange("p (h d) -> p h d", h=4))
                        else:
                            for g2 in range(2):
                                S.act(sqs[g2][:, 0:128], pb[:, g2 * 128:(g2 + 1) * 128], AF.Square,
                                      accum_out=sm[:, g2:g2 + 1])
                            S.ts(sm[:, 2:4], sm[:, 0:2], 1.0 / 128, ALU.mult, EPS, ALU.add)
                            S.act(sm[:, 2:4], sm[:, 2:4], AF.Sqrt)
                            S.recip(sm[:, 4:6], sm[:, 2:4])
                            for g2 in range(2):
                                S.stt(s_[:, g2 * 128:(g2 + 1) * 128], pb[:, g2 * 128:(g2 + 1) * 128],
                                      sm[:, 4 + g2:5 + g2], pp[:, o + P_CKNBC:o + P_CKNBC + 128],
                                      ALU.mult, ALU.mult)
                            S.cp(s_[:, 256:512], pb[:, 256:512], eng="act")
                            S.dma(ncc_out[seg, l, 0, :, tl:tl + 128, :].rearrange("h t d -> t h d"),
                                  s_[:, 0:256].rearrange("p (h d) -> p h d", h=2))
                            S.dma(ncc_out[seg, l, 1, :, tl:tl + 128, :].rearrange("h t d -> t h d"),
                                  s_[:, 256:512].rearrange("p (h d) -> p h d", h=2))
                            v16 = tk16[tb % 2]
                            S.cp(v16[:, 0:256], pb[:, 256:512])
                            S.dma(VCd[4 + tb], v16[:, 0:256].rearrange("p (h d) -> p h d", h=2))
            S.barrier()
            A.release(0)
            return BAt

        def phase_delta(l, DB):
            o = l * PL
            BAt = bat_t[:, :].rearrange("p (b c) -> p b c", b=16)
            keep = pp[:, P_KEEP:P_KEEP + 1]
            pad = A.f32(8 * 260).rearrange("p (s c) -> p s c", s=8)
            cv = A.f32(T)
            cv3 = cv.rearrange("p (s c) -> p s c", s=8)
            sqb = A.f32(T)
            rsb = A.f32(512)
            for ci in range(12):
                S.dma(pad[:, :, 2:258], AQ[ci].rearrange("p (s c) -> p s c", s=8))
                S.memset(pad[:, 0:1, 0:2], 0.0)
                S.memset(pad[:, 7:8, 258:260], 0.0)
                S.ts(pad[:, 1:8, 0:2], pad[:, 0:7, 256:258], keep, ALU.mult)
                S.ts(pad[:, 0:7, 258:260], pad[:, 1:8, 2:4], keep, ALU.mult)

                def w(j):
                    c0 = o + P_CONV + ci * 5 + j
                    return pp[:, c0:c0 + 1]
                S.ts(cv3, pad[:, :, 0:256], w(0), ALU.mult)
                for j in range(1, 5):
                    S.stt(cv3, pad[:, :, j:j + 256], w(j), cv3, ALU.mult, ALU.add)
                S.act(cv, cv, AF.Silu)
                if ci < 8:
                    S.act(sqb, cv, AF.Square)
                    for tt in range(4):
                        tsl = slice(tt * 512, (tt + 1) * 512)
                        pr = PS[tt % 2]
                        S.mm(pr[:, :], ones, sqb[:, tsl])
                        S.ts(rsb, pr[:, :], EPS, ALU.add)
                        S.act(rsb, rsb, AF.Sqrt)
                        S.recip(rsb, rsb)
                        if ci < 4:
                            S.stt(cv[:, tsl], cv[:, tsl], 128.0 ** -0.5, rsb, ALU.mult, ALU.mult)
                        else:
                            S.tt(cv[:, tsl], cv[:, tsl], rsb, ALU.mult)
                S.dma(AQ2[ci], cv)
            S.barrier()
            A.release(0)
            Bt = A.f32(128).rearrange("p (b c) -> p b c", b=16)
            Gt = A.f32(128).rearrange("p (b c) -> p b c", b=16)
            nA = A.f32(8)
            S.act(nA, pp[:, o + P_ALOG:o + P_ALOG + 8], AF.Exp)
            S.ts(nA, nA, -1.0, ALU.mult)
            S.act(Bt, BAt[:, :, 0:8], AF.Sigmoid)
            S.tt(Gt, BAt[:, :, 8:16], pp[:, o + P_DTB:o + P_DTB + 8].unsqueeze(1).to_broadcast([128, 16, 8]), ALU.add)
            S.act(Gt, Gt, AF.Exp)
            S.act(Gt, Gt, AF.Ln, bias=1.0)
            S.tt(Gt, Gt, nA.unsqueeze(1).to_broadcast([128, 16, 8]), ALU.mult)
            def B512():
                return A.f32(512)

            def v3(a):
                return a.rearrange("p (h j) -> p h j", h=4)

            def hs(a, h):
                return a[:, h * 128:(h + 1) * 128]
            otok = A.f32(16 * 512).rearrange("p (b x) -> p b x", b=16)
            S.memset(otok, 0.0)
            Irep = B512()
            S.tt(Irep, cst[:, C_LE:C_LE + 512], cst[:, C_GE:C_GE + 512], ALU.mult)

            def chain(dr, X, Y):
                Uin, Lst, Linc, Lstr = (C_LE, C_GT, C_GE, C_GT) if dr == 0 else (C_GE, C_LT, C_LE, C_LT)
                qkv = A.f32(12 * 128).rearrange("p (c t) -> p c t", c=12)
                E, t1, qk, N, NT, qkT, RT, Pa, PTa, Pb, PTb, vb, kdec, St = (B512() for _ in range(14))
                gm = E
                Dsb = E
                rhs2 = t1
                dl = Pa
                qss = PTa
                s0t = Pb
                sc12 = A.f32(12)
                nb = A.f32(4)
                bx = A.f32(4)
                S.memset(St, 0.0)
                order = range(16) if dr == 0 else range(15, -1, -1)
                for tb in order:
                    seg = tb // 2
                    is_start = (tb % 2 == 0) if dr == 0 else (tb % 2 == 1)
                    g4 = Gt[:, tb, dr * 4:dr * 4 + 4]
                    b4 = Bt[:, tb, dr * 4:dr * 4 + 4]

                    def bc(a):
                        return a.unsqueeze(2).to_broadcast([128, 4, 128])
                    S.dma(qkv, AQ2[:, :, tb * 128:(tb + 1) * 128].rearrange("c p t -> p c t"))
                    if is_start:
                        S.dma(v3(s0t), s0_d[l, dr, seg].rearrange("h k v -> k h v"))
                        S.stt(St, St, keep, s0t, ALU.mult, ALU.add)
                    S.tt(v3(gm), v3(cst[:, Lst:Lst + 512]), bc(g4), ALU.mult)
                    S.mm(X[:, :], cst[:, Uin:Uin + 128], gm)
                    S.mm(Y[:, 0:4], cst[:, Uin:Uin + 128], g4)
                    S.mm(Y[:, 4:8], cst[:, Lst:Lst + 128], g4)
                    S.mm(Y[:, 8:12], ones, g4)
                    yield 1
                    S.cp(Dsb, X[:, :])
                    S.act(E, Dsb, AF.Exp)
                    S.cp(sc12, Y[:, 0:12])
                    S.act(sc12, sc12, AF.Exp)
                    eg, er, ge = sc12[:, 0:4], sc12[:, 4:8], sc12[:, 8:12]
                    for h in range(4):
                        S.mm(hs(X, h), qkv[:, h, :], qkv[:, 4 + h, :])
                        S.mm(hs(Y, h), qkv[:, 4 + h, :], qkv[:, 4 + h, :])
                    yield 1
                    S.tt(t1, E, cst[:, Linc:Linc + 512], ALU.mult)
                    S.tt(qk, X[:, :], t1, ALU.mult)
                    S.tt(t1, E, cst[:, Lstr:Lstr + 512], ALU.mult)
                    S.tt(N, Y[:, :], t1, ALU.mult)
                    for h in range(4):
                        S.tr(hs(X, h), qkv[:, 4 + h, :], ident)
                        S.tr(hs(Y, h), qkv[:, 8 + h, :], ident)
                    S.ts(nb, b4, -1.0, ALU.mult)
                    S.tt(v3(N), v3(N), bc(nb), ALU.mult)
                    S.tt(bx, b4, eg, ALU.mult)
                    yield 1
                    S.tt(v3(kdec), v3(X[:, :]), bc(er), ALU.mult)
                    S.tt(v3(vb), v3(Y[:, :]), bc(b4), ALU.mult)
                    for h in range(4):
                        S.tr(hs(X, h), hs(N, h), ident)
                        S.tr(hs(Y, h), hs(qk, h), ident)
                    yield 1
                    S.cp(NT, X[:, :])
                    S.cp(qkT, Y[:, :])
                    S.tt(RT, NT, Irep, ALU.add)
                    P_, PT_ = N, NT
                    for stg in range(6):
                        Pn, PTn = (Pa, PTa) if stg % 2 == 0 else (Pb, PTb)
                        for h in range(4):
                            S.mm(hs(X, h), hs(PT_, h), hs(P_, h))
                        if stg < 5:
                            for h in range(4):
                                S.mm(hs(Y, h), hs(P_, h), hs(PT_, h))
                        yield 1
                        S.cp(Pn, X[:, :])
                        if stg < 5:
                            S.cp(PTn, Y[:, :])
                        for h in range(4):
                            S.mm(hs(X, h), hs(Pn, h), hs(RT, h))
                        yield 1
                        S.tt(RT, RT, X[:, :], ALU.add)
                        P_, PT_ = Pn, PTn
                    for h in range(4):
                        S.mm(hs(X, h), qkv[:, 4 + h, :], hs(St, h))
                        S.mm(hs(Y, h), qkv[:, h, :], hs(St, h))
                    yield 1
                    S.tt(v3(t1), v3(X[:, :]), bc(bx), ALU.mult)
                    S.tt(rhs2, vb, t1, ALU.subtract)
                    S.tt(v3(qss), v3(Y[:, :]), bc(eg), ALU.mult)
                    for h in range(4):
                        S.mm(hs(X, h), hs(RT, h), hs(rhs2, h))
                    yield 1
                    S.cp(dl, X[:, :])
                    for h in range(4):
                        S.mm(hs(Y, h), hs(qkT, h), hs(dl, h))
                        S.mm(hs(X, h), hs(kdec, h), hs(dl, h))
                    yield 1
                    S.tt(qss, Y[:, :], qss, ALU.add)
                    S.tt(otok[:, tb, :], otok[:, tb, :], qss, ALU.add)
                    for h in range(4):
                        S.stt(hs(St, h), hs(St, h), ge[:, h:h + 1], hs(X, h), ALU.mult, ALU.add)
                    if not is_start:
                        S.dma(na_out[seg, l, dr].rearrange("h k v -> k h v"), v3(St))
                    yield 1

            chains = [chain(0, DB[0], DB[1]), chain(1, DB[2], DB[3])]
            alive = [True, True]
            while any(alive):
                for ci_ in range(2):
                    if alive[ci_]:
                        try:
                            next(chains[ci_])
                        except StopIteration:
                            alive[ci_] = False
                yield 1
            yield 0
            ss = A.f32(8)
            junk = A.f32(128)
            on = A.f32(512)
            ag = [A.f32(512) for _ in range(2)]
            ob4 = [A.bf(512) for _ in range(2)]
            for tb in range(16):
                ot = otok[:, tb, :]
                for h in range(4):
                    S.act(junk, hs(ot, h), AF.Square, accum_out=ss[:, h:h + 1])
                S.ts(ss[:, 4:8], ss[:, 0:4], 1.0 / 128, ALU.mult, EPS, ALU.add)
                S.act(ss[:, 4:8], ss[:, 4:8], AF.Sqrt)
                S.recip(ss[:, 4:8], ss[:, 4:8])
                for h in range(4):
                    S.stt(hs(on, h), hs(ot, h), ss[:, 4 + h:5 + h], pp[:, o + P_ANORM:o + P_ANORM + 128],
                          ALU.mult, ALU.mult)
                pb = PS[tb % 2]
                for h in range(4):
                    S.tr(hs(pb, h), hs(on, h), ident)
                a_ = ag[tb % 2]
                S.dma(v3(a_), AG[:, :, tb * 128:(tb + 1) * 128].rearrange("c p t -> p c t"))
                ob = ob4[tb % 2]
                S.tt(ob, pb[:, :], a_, ALU.mult)
                S.dma(OTd[0:4, :, tb * 128:(tb + 1) * 128].rearrange("c p t -> p c t"), v3(ob))
            S.barrier()
            A.release(0)

        def layer_norm_fm(xt, gcol, bcol, bufs):
            sqs_, mean, msq, var = bufs
            pm, pq = PS[2], PS[3]
            for kc in range(KC):
                sqc = sqs_[kc % 2]
                S.act(sqc, xt[:, kc, :], AF.Square)
                S.mm(pm[:, :], ones, xt[:, kc, :], start=(kc == 0), stop=(kc == KC - 1))
                S.mm(pq[:, :], ones, sqc, start=(kc == 0), stop=(kc == KC - 1))
            S.ts(mean, pm[:, :], 1.0 / D, ALU.mult)
            S.tt(msq, mean, mean, ALU.mult)
            S.stt(var, pq[:, :], 1.0 / D, msq, ALU.mult, ALU.subtract)
            S.ts(var, var, EPS / (ALPHA * ALPHA), ALU.add)
            S.act(var, var, AF.Sqrt)
            S.recip(var, var)
            S.tt(xt, xt, mean.unsqueeze(1).to_broadcast([128, 16, 512]), ALU.subtract)
            S.tt(xt, xt, var.unsqueeze(1).to_broadcast([128, 16, 512]), ALU.mult)
            for kc in range(KC):
                S.act(xt[:, kc, :], xt[:, kc, :], AF.Identity, scale=gcol[:, kc:kc + 1], bias=bcol[:, kc:kc + 1])

        def phase_mix_out(l):
            o = l * PL
            ga1, sh2, sc2p = mdv(l, 2), mdv(l, 3), mdv(l, 4)
            lng = pp[:, o + P_LNG1:o + P_LNG1 + 16]
            lnb = pp[:, o + P_LNB1:o + P_LNB1 + 16]
            wo = A.bf(16 * D).rearrange("p (k n) -> p k n", k=16)
            for c4 in range(4):
                S.dma(wo[:, :, c4 * 512:(c4 + 1) * 512],
                      w_out[l, :, c4 * 512:(c4 + 1) * 512].rearrange("(k p) n -> p k n", p=128), q="pool")
            wrg = A.f32(320).rearrange("p (k c) -> p k c", k=16)
            S.dma(wrg, wrg_d[l])
            lg = A.f32(320).rearrange("p (b c) -> p b c", b=16)
            lgT = A.f32(512)
            ots = [A.bf(16 * 512).rearrange("p (k t) -> p k t", k=16) for _ in range(1)]
            xt = A.f32(16 * 512).rearrange("p (k t) -> p k t", k=16)
            h2b = A.bf(16 * 512).rearrange("p (k t) -> p k t", k=16)
            h2c = [A.f32(512) for _ in range(2)]
            lnb_ = ([A.f32(512) for _ in range(2)], A.f32(512), A.f32(512), A.f32(512))
            for tt in range(4):
                tsl = slice(tt * 512, (tt + 1) * 512)
                ot = ots[0]
                S.dma(ot, OTd[:, :, tsl].rearrange("c p t -> p c t"))
                S.dma(xt, XT[:, :, tsl].rearrange("c p t -> p c t"))
                for oc in range(KC):
                    pb = PS[oc % 2]
                    for kc in range(KC):
                        S.mm(pb[:, :], wo[:, kc, oc * 128:(oc + 1) * 128], ot[:, kc, :],
                             start=(kc == 0), stop=(kc == KC - 1))
                    S.stt(xt[:, oc, :], pb[:, :], ga1[:, oc:oc + 1], xt[:, oc, :], ALU.mult, ALU.add)
                layer_norm_fm(xt, lng, lnb, lnb_)
                S.dma(XT[:, :, tsl].rearrange("c p t -> p c t"), xt)
                pr = PS[4]
                for kc in range(KC):
                    hc = h2c[kc % 2]
                    S.act(hc, xt[:, kc, :], AF.Identity, scale=sc2p[:, kc:kc + 1], bias=sh2[:, kc:kc + 1])
                    S.cp(h2b[:, kc, :], hc)
                    S.mm(pr[0:20, :], wrg[:, kc, :], hc, start=(kc == 0), stop=(kc == KC - 1))
                S.dma(H2d[:, :, tsl].rearrange("c p t -> p c t"), h2b)
                S.cp(lgT[0:20, :], pr[0:20, :])
                pt = PS[5]
                for blk in range(4):
                    S.tr(pt[:, blk * 20:(blk + 1) * 20], lgT[0:20, blk * 128:(blk + 1) * 128], ident[0:20, 0:20])
                S.tt(lg[:, tt * 4:(tt + 1) * 4, :], pt[:, 0:80].rearrange("p (b c) -> p b c", b=4),
                     pp[:, o + P_BGR:o + P_BGR + 20].unsqueeze(1).to_broadcast([128, 4, 20]), ALU.add)
            def sm(n):
                return A.f32(n)
            gl = lg[:, :, 0:4]
            el4 = lg[:, :, 4:20].rearrange("p b (g e) -> p b g e", g=4)
            gmax, gsum, gv, m1, m2, d21, w1, w2 = (sm(16) for _ in range(8))
            ohg, gex, sel, oh1, msk, oh2, ge4, tmp4 = (sm(64).rearrange("p (b c) -> p b c", b=16) for _ in range(8))
            prod = sm(256).rearrange("p (b g e) -> p b g e", b=16, g=4)
            gates = sm(256)
            gates4 = gates.rearrange("p (b g e) -> p b g e", b=16, g=4)

            def b3(a):
                return a.unsqueeze(2).to_broadcast([128, 16, 4])
            S.red(gmax, gl, ALU.max)
            S.tt(ohg, gl, b3(gmax), ALU.is_equal)
            S.tt(gex, gl, b3(gmax), ALU.subtract)
            S.act(gex, gex, AF.Exp)
            S.red(gsum, gex, ALU.add)
            S.recip(gv, gsum)
            S.tt(prod, el4, ohg.unsqueeze(3).to_broadcast([128, 16, 4, 4]), ALU.mult)
            S.red(sel, prod.rearrange("p b g e -> p b e g"), ALU.add)
            S.red(m1, sel, ALU.max)
            S.tt(oh1, sel, b3(m1), ALU.is_equal)
            S.stt(msk, oh1, -1.0e30, sel, ALU.mult, ALU.add)
            S.red(m2, msk, ALU.max)
            S.tt(oh2, msk, b3(m2), ALU.is_equal)
            S.tt(d21, m2, m1, ALU.subtract)
            S.act(d21, d21, AF.Exp)
            S.ts(w1, d21, 1.0, ALU.add)
            S.recip(w1, w1)
            S.tt(w2, d21, w1, ALU.mult)
            S.tt(w1, w1, gv, ALU.mult)
            S.tt(w2, w2, gv, ALU.mult)
            S.tt(ge4, oh1, b3(w1), ALU.mult)
            S.tt(tmp4, oh2, b3(w2), ALU.mult)
            S.tt(ge4, ge4, tmp4, ALU.add)
            S.tt(gates4, ohg.unsqueeze(3).to_broadcast([128, 16, 4, 4]),
                 ge4.unsqueeze(2).to_broadcast([128, 16, 4, 4]), ALU.mult)
            gT = A.f32(T)
            for tb in range(16):
                pb = PS[6 + (tb // 4) % 2]
                S.tr(pb[0:16, (tb % 4) * 128:(tb % 4 + 1) * 128], gates[:, tb * 16:(tb + 1) * 16], ident)
                if tb % 4 == 3:
                    S.cp(gT[0:16, (tb // 4) * 512:(tb // 4 + 1) * 512], pb[0:16, :])
            gbs = [A.f32(512) for _ in range(3)]
            for e in range(16):
                for tt in range(4):
                    tsl = slice(tt * 512, (tt + 1) * 512)
                    pb = PS[(e * 4 + tt) % 2]
                    S.mm(pb[:, :], cst[0:16, C_SEL + e * 128:C_SEL + (e + 1) * 128], gT[0:16, tsl])
                    gb = gbs[(e * 4 + tt) % 3]
                    S.cp(gb, pb[:, :])
                    S.dma(GBd[e, :, tsl], gb)
            S.barrier()
            A.release(0)

        def phase_moe(l):
            o = l * PL
            ga2 = mdv(l, 5)
            lng = pp[:, o + P_LNG2:o + P_LNG2 + 16]
            lnb = pp[:, o + P_LNB2:o + P_LNB2 + 16]
            h2b = A.bf(16 * 512).rearrange("p (k t) -> p k t", k=16)
            yacc = A.f32(16 * 512).rearrange("p (k t) -> p k t", k=16)
            wgs = [A.bf(16 * 512).rearrange("p (k f) -> p k f", k=16) for _ in range(2)]
            wus = [A.bf(16 * 512).rearrange("p (k f) -> p k f", k=16) for _ in range(2)]
            wds = [A.bf(4 * D).rearrange("p (c d) -> p c d", c=4) for _ in range(2)]
            gbs = [A.f32(512) for _ in range(2)]
            sgs = [A.f32(512) for _ in range(2)]
            tms = [A.f32(512) for _ in range(2)]
            actb = [[A.bf(512) for _ in range(4)] for _ in range(2)]
            xcs = [A.f32(512) for _ in range(2)]
            lnb_ = (sgs, tms[0], tms[1], gbs[0])
            ui = 0
            for tm in range(4):
                tsl = slice(tm * 512, (tm + 1) * 512)
                S.dma(h2b, H2d[:, :, tsl].rearrange("c p t -> p c t"))
                for e in range(int(os.environ.get("DBG_NEXP", "16"))):
                    gb = gbs[e % 2]
                    S.dma(gb, GBd[e, :, tsl])
                    for hf in range(2):
                        wg, wu, wd = wgs[ui % 2], wus[ui % 2], wds[ui % 2]
                        ab = actb[ui % 2]
                        ui += 1
                        fsl = slice(hf * 512, (hf + 1) * 512)
                        S.dma(wg, w_gate[l, e, :, fsl].rearrange("(k p) f -> p k f", p=128), q="pool")
                        S.dma(wu, w_up[l, e, :, fsl].rearrange("(k p) f -> p k f", p=128), q="pool")
                        S.dma(wd, w_down[l, e, fsl, :].rearrange("(c p) d -> p c d", p=128), q="pool")
                        for fc in range(4):
                            pg, pu = PS[fc % 2], PS[2 + fc % 2]
                            for kc in range(KC):
                                S.mm(pg[:, :], wg[:, kc, fc * 128:(fc + 1) * 128], h2b[:, kc, :],
                                     start=(kc == 0), stop=(kc == KC - 1))
                            for kc in range(KC):
                                S.mm(pu[:, :], wu[:, kc, fc * 128:(fc + 1) * 128], h2b[:, kc, :],
                                     start=(kc == 0), stop=(kc == KC - 1))
                            sg = sgs[fc % 2]
                            S.act(sg, pg[:, :], AF.Silu)
                            tmb = tms[fc % 2]
                            S.tt(tmb, pu[:, :], sg, ALU.mult)
                            S.tt(ab[fc], tmb, gb, ALU.mult)
                        for oc in range(KC):
                            py = PS[4 + oc % 4]
                            for fc in range(4):
                                S.mm(py[:, :], wd[:, fc, oc * 128:(oc + 1) * 128], ab[fc],
                                     start=(fc == 0), stop=(fc == 3))
                            if e == 0 and hf == 0:
                                S.cp(yacc[:, oc, :], py[:, :])
                            else:
                                S.tt(yacc[:, oc, :], yacc[:, oc, :], py[:, :], ALU.add)
                for oc in range(KC):
                    xc = xcs[oc % 2]
                    S.dma(xc, XT[oc, :, tsl])
                    S.stt(yacc[:, oc, :], yacc[:, oc, :], ga2[:, oc:oc + 1], xc, ALU.mult, ALU.add)
                layer_norm_fm(yacc, lng, lnb, lnb_)
                S.dma(XT[:, :, tsl].rearrange("c p t -> p c t"), yacc)
            S.barrier()
            A.release(0)

        def phase_attn(l, coop=False):
            import math
            o = l * PL
            lam_init = 0.8 - 0.6 * math.exp(-0.3 * l)
            lamt = misc[:, 32:40]
            bl = pp[:, o + P_BLAM:o + P_BLAM + 256]
            prod = A.f32(128)
            S.tt(prod[:, 0:64], bl[:, 0:64], bl[:, 64:128], ALU.mult)
            S.tt(prod[:, 64:128], bl[:, 128:192], bl[:, 192:256], ALU.mult)
            S.red(lamt[:, 0:2], prod.rearrange("p (a b) -> p a b", a=2), ALU.add)
            S.act(lamt[:, 2:4], lamt[:, 0:2], AF.Exp)
            S.tt(lamt[:, 4:5], lamt[:, 2:3], lamt[:, 3:4], ALU.subtract)
            S.ts(lamt[:, 5:6], lamt[:, 4:5], -1.0, ALU.mult, -lam_init, ALU.add)
            neglam = lamt[:, 5:6]
            kTs = [A.bf(2560) for _ in range(2)]
            kT2s = [A.bf(2560) for _ in range(2)]
            for i_ in range(2):
                S.memset(kTs[i_][64:128, :], 0.0)
                S.memset(kT2s[i_][0:64, :], 0.0)
            qTs = [A.bf(2 * T) for _ in range(2)]
            vvs = [A.bf(2560).rearrange("p (b d) -> p b d", b=20) for _ in range(2)]
            pts = [A.bf(512) for _ in range(4)]
            nb_ = 1 if coop else 2
            rds = [A.f32(512) for _ in range(nb_)] * (2 // nb_)
            ons = [A.f32(512) for _ in range(nb_)] * (2 // nb_)
            dfs = [A.f32(256) for _ in range(nb_)] * (2 // nb_)
            sq2 = [A.f32(256) for _ in range(nb_)] * (2 // nb_)
            rs2 = [A.f32(256) for _ in range(nb_)] * (2 // nb_)
            obs = [A.bf(512) for _ in range(2)]
            cnt = 0
            unit = 0
            for si in range(8):
                isB = si < 4
                if os.environ.get("DBG_ATT") and ("B" if isB else "C") not in os.environ["DBG_ATT"]:
                    continue
                if os.environ.get("DBG_ATTS") and str(si) not in os.environ["DBG_ATTS"]:
                    continue
                kT = kTs[si % 2]
                qT = qTs[si % 2]
                vv = vvs[si % 2]
                if isB:
                    h = si
                    kT2 = kT2s[si % 2]
                    S.dma(kT[0:64, :], KBd[h, 0:64, :])
                    S.dma(kT2[64:128, :], KBd[h, 64:128, :])
                    S.dma(qT[:, 0:T], QBd[h])
                    S.dma(vv, VBd[:, :, h, :].rearrange("b p d -> p b d"))
                    scale = 0.125
                else:
                    g2, pr_ = divmod(si - 4, 2)
                    h0 = g2 * 4 + pr_ * 2
                    S.dma(kT, KCd[g2])
                    S.dma(qT.rearrange("p (c t) -> p c t", c=2), QCd[h0:h0 + 2].rearrange("c p t -> p c t"))
                    S.dma(vv, VCd[:, :, g2, :].rearrange("b p d -> p b d"))
                    scale = 128.0 ** -0.5
                q3 = qT.rearrange("p (c t) -> p c t", c=2)
                for qs in range(int(os.environ.get("DBG_ATTQ", "8"))):
                    O = PS[4] if coop else PS[unit % 2]
                    DN = PS[5] if coop else PS[2 + unit % 2]
                    unit += 1
                    qsl = slice(qs * 256, (qs + 1) * 256)

                    def smm(kb, sb):
                        ksl = slice(kb * 128, (kb + 1) * 128)
                        if isB:
                            S.mm(sb[:, 0:256], kT[:, ksl], qT[:, qsl])
                            S.mm(sb[:, 256:512], kT2[:, ksl], qT[:, qsl])
                        else:
                            S.mm(sb[:, :].rearrange("p (c t) -> p c t", c=2), kT[:, ksl], q3[:, :, qsl])

                    sbs = {}
                    def sbank(i_):
                        return PS[6 + i_ % 2] if coop else PS[4 + i_ % 4]
                    sbs[0] = sbank(cnt)
                    smm(0, sbs[0])
                    for kb in range(20):
                        if kb + 1 < 20:
                            sbs[kb + 1] = sbank(cnt + 1)
                            smm(kb + 1, sbs[kb + 1])
                        pt = pts[cnt % 4]
                        S.act(pt, sbs[kb][:, :], AF.Exp, scale=scale, bias=mask[:, qs * 20 + kb:qs * 20 + kb + 1])
                        S.mm(O[:, :], vv[:, kb, :], pt, start=(kb == 0), stop=(kb == 19))
                        S.mm(DN[:, :], onesb, pt, start=(kb == 0), stop=(kb == 19))
                        cnt += 1
                        yield 1
                    u2 = unit % 2
                    rd = rds[u2]
                    S.cp(rd, DN[:, :])
                    S.recip(rd, rd)
                    if isB and os.environ.get("DBG_BFIN") == "0":
                        pass
                    elif isB:
                        cut = int(os.environ.get("DBG_BFIN", "9"))
                        on = ons[u2]
                        S.tt(on, O[:, :], rd, ALU.mult)
                        df = dfs[u2]
                        S.stt(df, on[:, 256:512], neglam, on[:, 0:256], ALU.mult, ALU.add)
                        if cut >= 2:
                            sq = sq2[u2]
                            S.act(sq, df, AF.Square)
                            S.mm(DN[:, 0:256], ones, sq)
                        if cut >= 3:
                            rs = rs2[u2]
                            S.ts(rs, DN[:, 0:256], 1.0 / 128, ALU.mult, EPS, ALU.add)
                            S.act(rs, rs, AF.Sqrt)
                            S.recip(rs, rs)
                        if cut >= 4:
                            S.stt(df, df, pp[:, o + P_BNORM:o + P_BNORM + 1], rs, ALU.mult, ALU.mult)
                            ob = obs[u2]
                            S.ts(ob[:, 0:256], df, 1.0 - lam_init, ALU.mult)
                        if cut >= 5:
                            S.dma(OTd[4 + h, :, qsl], ob[:, 0:256])
                    else:
                        ob = obs[u2]
                        S.tt(ob, O[:, :], rd, ALU.mult)
                        S.dma(OTd[8 + h0:8 + h0 + 2, :, qsl].rearrange("c p t -> p c t"),
                              ob.rearrange("p (c t) -> p c t", c=2))
            if not coop:
                S.barrier()
                A.release(0)

        def run_gen(g):
            for _ in g:
                pass

        def phase_mixers(l):
            dg = phase_delta(l, [PS[0], PS[1], PS[2], PS[3], PS[0], PS[1], PS[2], PS[3]])
            ag = None
            d_loop_done = False
            a_done = False
            while True:
                if not d_loop_done:
                    r = next(dg)
                    if r == 0:
                        d_loop_done = True
                if ag is None:
                    ag = phase_attn(l, coop=True)
                if not a_done:
                    for _ in range(3):
                        try:
                            next(ag)
                        except StopIteration:
                            a_done = True
                            break
                if d_loop_done and a_done:
                    break
            run_gen(dg)

        if stages == "all":
            for l in range(L):
                phase_proj(l)
                phase_mixers(l)
                phase_mix_out(l)
                phase_moe(l)
        elif stages == "dbgM":
            phase_proj(0)
            phase_mixers(0)
            phase_mix_out(0)
            dbg_x = nc.dram_tensor("dbg_x", [16, 128, T], F32, kind="ExternalOutput").ap()
            dbg_gb = nc.dram_tensor("dbg_gb", [16, 128, T], F32, kind="ExternalOutput").ap()
            dbg_h2 = nc.dram_tensor("dbg_h2", [16, 128, T], BF, kind="ExternalOutput").ap()
            S.dram_names.update(("dbg_x", "dbg_gb", "dbg_h2"))
            S.dma(dbg_x, XT)
            S.dma(dbg_gb, GBd)
            S.dma(dbg_h2, H2d)
        elif stages.startswith("dbgP"):
            phase_proj(0)
            if "X" in stages:
                phase_mixers(0)
            else:
                if "A" in stages:
                    run_gen(phase_delta(0, PS))
                if "T" in stages:
                    run_gen(phase_attn(0))
            dbg_ot = nc.dram_tensor("dbg_ot", [16, 128, T], BF, kind="ExternalOutput").ap()
            S.dram_names.add("dbg_ot")
            S.dma(dbg_ot, OTd)
        elif stages.startswith("dbg"):
            phase_proj(0, upto=int(stages[3:]))

        xfs = [A.f32(D).rearrange("p (k t) -> p k t", k=16) for _ in range(2)]
        ysg = [A.f32(D) for _ in range(2)]
        for tb in range(16):
            xf = xfs[tb % 2]
            S.dma(xf, XT[:, :, tb * 128:(tb + 1) * 128].rearrange("k p t -> p k t"))
            yo = ysg[tb % 2]
            for k4 in range(4):
                pb = PS[(tb * 4 + k4) % 4]
                for j in range(4):
                    kc = k4 * 4 + j
                    S.tr(pb[:, j * 128:(j + 1) * 128], xf[:, kc, :], ident)
                S.cp(yo[:, k4 * 512:(k4 + 1) * 512], pb[:, :], eng=("act" if k4 % 2 else "dve"))
            S.dma(y_out[tb * 128:(tb + 1) * 128, :], yo)

        S.emit(sems, dsems)
    return nc


def kernel(**inp):
    inp = {k: np.asarray(v) for k, v in inp.items()}
    nc = build_program()
    cst = _consts()
    in_maps = []
    wrg = np.ascontiguousarray(
        np.concatenate([inp["w_group"], inp["w_router"]], axis=2).reshape(L, 16, 128, 20).transpose(0, 2, 1, 3))
    shared = dict(cst=cst, w_ada=inp["w_ada"], w_in=inp["w_in"], w_out=inp["w_out"], wrg=wrg,
                  w_gate=inp["w_gate"], w_up=inp["w_up"], w_down=inp["w_down"])
    for c in range(NCORES):
        sample = c < 4
        m = dict(shared)
        if sample:
            m["x"] = np.ascontiguousarray(inp["x_sample"][c])
            cond = inp["c"][c]
            s0 = np.zeros((L, 2, 8, 4, 128, 128), np.float32)
            s0[:, 0, 0] = inp["state_a"][c, :, 0]
            s0[:, 1, 7] = inp["state_a"][c, :, 1]
            m["cache_b"] = np.ascontiguousarray(inp["cache_b_kv"][c])
            m["cache_c"] = np.ascontiguousarray(inp["cache_c_kv"][c])
        else:
            p0 = (c - 4) * 8
            m["x"] = np.ascontiguousarray(inp["x_prompt"][p0:p0 + 8].reshape(T, D))
            cond = inp["c_ctx"]
            s0 = np.zeros((L, 2, 8, 4, 128, 128), np.float32)
            m["cache_b"] = np.zeros((L, 2, 4, 512, 128), np.float32)
            m["cache_c"] = np.zeros((L, 2, 2, 512, 128), np.float32)
        m["s0"] = s0
        m["pp"] = _params(inp, cond, sample)
        m["maskb"] = _mask(sample)
        m["rope"] = _rope_tables(sample)
        in_maps.append(m)
    res = run_bass_kernel_spmd(nc, in_maps, core_ids=list(range(NCORES)))
    r = res.results
    y_s = np.stack([r[c]["y"] for c in range(4)], 0).astype(np.float32)
    y_p = np.concatenate([r[c]["y"].reshape(8, 256, D) for c in range(4, 8)], 0).astype(np.float32)
    new_a = np.concatenate([r[c]["new_a"] for c in range(4, 8)], 0).astype(np.float32)
    new_b = np.concatenate([r[c]["new_b"] for c in range(4, 8)], 0).astype(np.float32)
    new_c = np.concatenate([r[c]["new_c"] for c in range(4, 8)], 0).astype(np.float32)
    return (y_p, y_s, new_a, new_b, new_c)
```
